# Optimizing a Trainium2 kernel written in Bass

```python
import math
import jax, jax.numpy as jnp
from jax import lax
import numpy as np

D_MODEL = 1024
BATCH = 4
SEQ = 8192
DEPTH = 1

D_SSM = D_MODEL // 2
D_CONV = D_MODEL - D_SSM
SSM_GROUP = 16
N_SSM_GROUPS = D_SSM // SSM_GROUP
SSM_STATE = 64
CONV_WIDTH = 31
D_IN = D_SSM + 2 * D_CONV
N_EXPERTS = 32
TOP_K = 4
D_FF = D_MODEL
SWIGLU_ALPHA = 1.702
SWIGLU_LIMIT = 7.0
ROW_BLOCK = 128
DT_MIN = 1e-3
DT_MAX = 1e-1
RMS_EPS = 1e-6
LN_EPS = 1e-5

kernel_name = "hybrid_s5_conformer_moe_adaln"


def rmsnorm(x, g, eps=RMS_EPS):
    xf = x.astype(jnp.float32)
    y = xf * lax.rsqrt(jnp.mean(xf * xf, axis=-1, keepdims=True) + eps)
    return (y * g.astype(jnp.float32)).astype(x.dtype)


def layernorm(x, g, b, eps=LN_EPS):
    xf = x.astype(jnp.float32)
    mu = jnp.mean(xf, axis=-1, keepdims=True)
    xc = xf - mu
    y = xc * lax.rsqrt(jnp.mean(xc * xc, axis=-1, keepdims=True) + eps)
    return (y * g.astype(jnp.float32) + b.astype(jnp.float32)).astype(x.dtype)


def s5_mixer(u, lam_re, lam_im, log_dt, b_re, b_im, c_re, c_im, d_skip, w_glu, b_glu):
    bsz, seq, _ = u.shape
    dtype = u.dtype
    f32 = jnp.float32
    ug = u.astype(f32).reshape(bsz, seq, N_SSM_GROUPS, SSM_GROUP)
    lr = lam_re.astype(f32)
    li = lam_im.astype(f32)
    dt = jnp.exp(log_dt.astype(f32))[:, None]
    mag = jnp.exp(lr * dt)
    ab_re = mag * jnp.cos(li * dt)
    ab_im = mag * jnp.sin(li * dt)
    den = lr * lr + li * li
    nr = ab_re - 1.0
    q_re = (nr * lr + ab_im * li) / den
    q_im = (ab_im * lr - nr * li) / den
    br = b_re.astype(f32)
    bi = b_im.astype(f32)
    bb_re = q_re[..., None] * br - q_im[..., None] * bi
    bb_im = q_re[..., None] * bi + q_im[..., None] * br
    bu_re = jnp.einsum('blgh,gph->blgp', ug, bb_re)
    bu_im = jnp.einsum('blgh,gph->blgp', ug, bb_im)
    a_re = jnp.broadcast_to(ab_re, bu_re.shape)
    a_im = jnp.broadcast_to(ab_im, bu_im.shape)

    def combine(e1, e2):
        a1r, a1i, b1r, b1i = e1
        a2r, a2i, b2r, b2i = e2
        return (a1r * a2r - a1i * a2i,
                a1r * a2i + a1i * a2r,
                a2r * b1r - a2i * b1i + b2r,
                a2r * b1i + a2i * b1r + b2i)

    _, _, xr, xi = lax.associative_scan(combine, (a_re, a_im, bu_re, bu_im), axis=1)
    y = (jnp.einsum('blgp,ghp->blgh', xr, c_re.astype(f32))
         - jnp.einsum('blgp,ghp->blgh', xi, c_im.astype(f32)))
    y = y + d_skip.astype(f32).reshape(N_SSM_GROUPS, SSM_GROUP) * ug
    y = jax.nn.gelu(y.reshape(bsz, seq, D_SSM)).astype(dtype)
    return y * jax.nn.sigmoid(y @ w_glu + b_glu)


def conformer_conv(v, g, conv_w, conv_b, ln_g, ln_b):
    z = v * jax.nn.sigmoid(g)
    kern = conv_w[:, None, :].astype(z.dtype)
    z = lax.conv_general_dilated(z, kern, window_strides=(1,),
                                 padding=[(CONV_WIDTH - 1, 0)],
                                 dimension_numbers=('NWC', 'WIO', 'NWC'),
                                 feature_group_count=D_CONV) + conv_b
    return jax.nn.silu(layernorm(z, ln_g, ln_b))


def moe_ffn(h, router_w, router_b, w_gate, b_gate, w_up, b_up, w_down, b_down):
    bsz, seq, d = h.shape
    n_tok = bsz * seq
    hf = h.reshape(n_tok, d)
    logits = (hf @ router_w + router_b).astype(jnp.float32)
    top_val, top_idx = lax.top_k(logits, TOP_K)
    top_w = jax.nn.softmax(top_val, axis=-1).astype(h.dtype)
    n_assign = n_tok * TOP_K
    n_blocks = -(-n_assign // ROW_BLOCK) + N_EXPERTS
    n_rows = n_blocks * ROW_BLOCK
    flat_e = top_idx.reshape(-1)
    flat_tok = jnp.repeat(jnp.arange(n_tok, dtype=jnp.int32), TOP_K)
    flat_w = top_w.reshape(-1)
    order = jnp.argsort(flat_e)
    se = flat_e[order]
    counts = jnp.bincount(flat_e, length=N_EXPERTS)
    padded = (counts + ROW_BLOCK - 1) // ROW_BLOCK * ROW_BLOCK
    start = jnp.cumsum(counts) - counts
    pend = jnp.cumsum(padded)
    pstart = pend - padded
    dest = pstart[se] + (jnp.arange(n_assign, dtype=jnp.int32) - start[se])
    row_tok = jnp.full((n_rows,), n_tok, jnp.int32).at[dest].set(flat_tok[order])
    row_w = jnp.zeros((n_rows,), h.dtype).at[dest].set(flat_w[order])
    block_e = jnp.minimum(
        jnp.searchsorted(pend, jnp.arange(n_blocks, dtype=jnp.int32) * ROW_BLOCK, side='right'),
        N_EXPERTS - 1)
    h_pad = jnp.concatenate([hf, jnp.zeros((1, d), hf.dtype)], axis=0)
    xs = h_pad[row_tok].reshape(n_blocks, ROW_BLOCK, d)

    def expert_block(args):
        xb, e = args
        gate = jnp.minimum(xb @ w_gate[e] + b_gate[e], SWIGLU_LIMIT)
        up = jnp.clip(xb @ w_up[e] + b_up[e], -SWIGLU_LIMIT, SWIGLU_LIMIT)
        glu = gate * jax.nn.sigmoid(SWIGLU_ALPHA * gate)
        return ((up + 1.0) * glu) @ w_down[e] + b_down[e]

    ys = lax.map(expert_block, (xs, block_e)).reshape(n_rows, d)
    out = jax.ops.segment_sum(ys * row_w[:, None], row_tok, num_segments=n_tok + 1)[:n_tok]
    return out.reshape(bsz, seq, d)


def setup_inputs(seed: int = 0) -> dict:
    key = jax.random.key(seed)
    ks = jax.random.split(key, 32)
    f32 = jnp.float32

    def nrm(k, shape, scale):
        return jax.random.normal(k, shape, f32) * scale

    L, D, G, P, H = DEPTH, D_MODEL, N_SSM_GROUPS, SSM_STATE, SSM_GROUP
    E, F = N_EXPERTS, D_FF
    lam_im_base = jnp.broadcast_to(math.pi * jnp.arange(P, dtype=f32), (L, G, P))
    return {
        "x": nrm(ks[0], (BATCH, SEQ, D), 1.0),
        "c": nrm(ks[1], (BATCH, D), 1.0),
        "ada_w": nrm(ks[2], (L, D, 6 * D), 0.5 * D ** -0.5),
        "ada_b": nrm(ks[3], (L, 6 * D), 0.01),
        "norm1_g": 1.0 + nrm(ks[4], (L, D), 0.02),
        "w_in": nrm(ks[5], (L, D, D_IN), D ** -0.5),
        "lam_re": -0.5 + nrm(ks[6], (L, G, P), 0.01),
        "lam_im": lam_im_base + nrm(ks[7], (L, G, P), 0.01),
        "log_dt": jax.random.uniform(ks[8], (L, G), f32, math.log(DT_MIN), math.log(DT_MAX)),
        "b_re": nrm(ks[9], (L, G, P, H), (2 * H) ** -0.5),
        "b_im": nrm(ks[10], (L, G, P, H), (2 * H) ** -0.5),
        "c_re": nrm(ks[11], (L, G, H, P), (2 * P) ** -0.5),
        "c_im": nrm(ks[12], (L, G, H, P), (2 * P) ** -0.5),
        "d_skip": nrm(ks[13], (L, D_SSM), 1.0),
        "w_glu": nrm(ks[14], (L, D_SSM, D_SSM), D_SSM ** -0.5),
        "b_glu": nrm(ks[15], (L, D_SSM), 0.01),
        "conv_w": nrm(ks[16], (L, CONV_WIDTH, D_CONV), CONV_WIDTH ** -0.5),
        "conv_b": nrm(ks[17], (L, D_CONV), 0.01),
        "ln_g": 1.0 + nrm(ks[18], (L, D_CONV), 0.02),
        "ln_b": nrm(ks[19], (L, D_CONV), 0.01),
        "out_norm_g": 1.0 + nrm(ks[20], (L, D), 0.02),
        "w_out": nrm(ks[21], (L, D, D), D ** -0.5),
        "norm2_g": 1.0 + nrm(ks[22], (L, D), 0.02),
        "router_w": nrm(ks[23], (L, D, E), D ** -0.5),
        "router_b": nrm(ks[24], (L, E), 0.01),
        "w_gate": nrm(ks[25], (L, E, D, F), D ** -0.5),
        "b_gate": nrm(ks[26], (L, E, F), 0.01),
        "w_up": nrm(ks[27], (L, E, D, F), D ** -0.5),
        "b_up": nrm(ks[28], (L, E, F), 0.01),
        "w_down": nrm(ks[29], (L, E, F, D), F ** -0.5),
        "b_down": nrm(ks[30], (L, E, D), 0.01),
        "final_g": 1.0 + nrm(ks[31], (D,), 0.02),
    }


def reference(x, c, ada_w, ada_b, norm1_g, w_in, lam_re, lam_im, log_dt, b_re, b_im,
              c_re, c_im, d_skip, w_glu, b_glu, conv_w, conv_b, ln_g, ln_b, out_norm_g,
              w_out, norm2_g, router_w, router_b, w_gate, b_gate, w_up, b_up, w_down,
              b_down, final_g):
    cond = jax.nn.silu(c)
    for i in range(DEPTH):
        mod = cond @ ada_w[i] + ada_b[i]
        sh1, sc1, g1, sh2, sc2, g2 = [m[:, None, :] for m in jnp.split(mod, 6, axis=-1)]
        h = rmsnorm(x, norm1_g[i]) * (1.0 + sc1) + sh1
        proj = h @ w_in[i]
        u = proj[..., :D_SSM]
        v = proj[..., D_SSM:D_SSM + D_CONV]
        gt = proj[..., D_SSM + D_CONV:]
        y_ssm = s5_mixer(u, lam_re[i], lam_im[i], log_dt[i], b_re[i], b_im[i], c_re[i],
                         c_im[i], d_skip[i], w_glu[i], b_glu[i])
        y_conv = conformer_conv(v, gt, conv_w[i], conv_b[i], ln_g[i], ln_b[i])
        merged = jnp.concatenate([rmsnorm(y_ssm, out_norm_g[i, :D_SSM]),
                                  rmsnorm(y_conv, out_norm_g[i, D_SSM:])], axis=-1)
        x = x + g1 * (merged @ w_out[i])
        h = rmsnorm(x, norm2_g[i]) * (1.0 + sc2) + sh2
        x = x + g2 * moe_ffn(h, router_w[i], router_b[i], w_gate[i], b_gate[i], w_up[i],
                             b_up[i], w_down[i], b_down[i])
    return rmsnorm(x, final_g)
```

```python
import math
import numpy as np
from contextlib import ExitStack
import concourse.bass as bass
import concourse.mybir as mybir
from concourse.bass_utils import run_bass_kernel_spmd

F32 = mybir.dt.float32
BF16 = mybir.dt.bfloat16
I32 = mybir.dt.int32
AF = mybir.ActivationFunctionType
ALU = mybir.AluOpType

D = 1024
NTOK = 4096
NST = 8
NEXP = 32
TS = 128
TWO_PI = 2.0 * math.pi
RS = 384
NSLOT = (NTOK * 4) // RS + 32


class Buf:
    __slots__ = ("name", "w", "r", "sem", "semv")

    def __init__(self, name):
        self.name = name
        self.w = None
        self.r = []
        self.sem = None
        self.semv = 0


class Sched:
    SEM_LIMIT = 30000

    def __init__(self, nc, stack):
        self.nc = nc
        self.stack = stack
        self.engs = {"pe": nc.tensor, "act": nc.scalar, "dve": nc.vector, "pool": nc.gpsimd, "sp": nc.sync}
        self.esem = {}
        self.ecnt = {}
        self.waited = {}
        self.nsem = 0
        self.pesems = set()
        self.dsems = {}
        self.allsems = []
        for e in ("pe", "act", "dve", "pool"):
            self.esem[e] = self.new_sem(e)
            self.ecnt[e] = 0
        self.pesems.add(id(self.esem["pe"]))

    def new_sem(self, name):
        self.nsem += 1
        return self.stack.enter_context(self.nc.semaphore(f"s{self.nsem}_{name}"))

    def barrier(self):
        evs = [(self.esem[e], self.ecnt[e]) for e in ("pe", "act", "dve", "pool") if self.ecnt[e] > 0]
        evs += list(self.dsems.values())
        for e in ("pe", "act", "dve", "pool", "sp"):
            for ev in evs:
                if e != "sp" and ev[0] is self.esem.get(e):
                    continue
                self._wait(e, ev)

    def _wait(self, e, ev):
        if ev is None:
            return
        sem, val = ev
        if e == "pe" and id(sem) in self.pesems:
            return
        key = (e, id(sem))
        if self.waited.get(key, 0) >= val:
            return
        self.waited[key] = val
        self.engs[e].wait_ge(sem, val)

    def _deps(self, e, reads, writes):
        for b in reads:
            self._wait(e, b.w)
        for b in writes:
            self._wait(e, b.w)
            for ev in b.r:
                self._wait(e, ev)

    def _commit(self, ev, reads, writes):
        for b in reads:
            b.r.append(ev)
            if len(b.r) > 64:
                last = {}
                for s, v in b.r:
                    if id(s) not in last or last[id(s)][1] < v:
                        last[id(s)] = (s, v)
                b.r = list(last.values())
        for b in writes:
            b.w = ev
            b.r = []

    def op(self, e, fn, reads=(), writes=()):
        self._deps(e, reads, writes)
        ins = fn(self.engs[e])
        if self.ecnt[e] >= self.SEM_LIMIT:
            self.esem[e] = self.new_sem(e)
            self.ecnt[e] = 0
            if e == "pe":
                self.pesems.add(id(self.esem[e]))
        self.ecnt[e] += 1
        ev = (self.esem[e], self.ecnt[e])
        ins.then_inc(self.esem[e], 1)
        self._commit(ev, reads, writes)
        return ins

    def dma(self, q, out, in_, reads=(), writes=(), key=None):
        self._deps(q, reads, writes)
        kb = key if key is not None else (writes[0] if writes else reads[0])
        if kb.sem is None:
            kb.sem = self.new_sem("d_" + kb.name)
            kb.semv = 0
        ins = self.engs[q].dma_start(out=out, in_=in_)
        kb.semv += 16
        ins.then_inc(kb.sem, 16)
        self.dsems[id(kb.sem)] = (kb.sem, kb.semv)
        self._commit((kb.sem, kb.semv), reads, writes)
        return ins

    def idma(self, out, out_off, in_, in_off, bound, reads=(), writes=(), key=None):
        self._deps("pool", reads, writes)
        kb = key
        if kb.sem is None:
            kb.sem = self.new_sem("d_" + kb.name)
            kb.semv = 0
        oo = bass.IndirectOffsetOnAxis(ap=out_off, axis=0) if out_off is not None else None
        io = bass.IndirectOffsetOnAxis(ap=in_off, axis=0) if in_off is not None else None
        try:
            ins = self.nc.gpsimd.indirect_dma_start(out=out, out_offset=oo, in_=in_, in_offset=io)
        except Exception:
            print("IDMA FAIL", out, in_, out_off, in_off, bound)
            raise
        kb.semv += 16
        ins.then_inc(kb.sem, 16)
        self.dsems[id(kb.sem)] = (kb.sem, kb.semv)
        self._commit((kb.sem, kb.semv), reads, writes)
        return ins

    def wait_all(self, e, bufs):
        for b in bufs:
            self._wait(e, b.w)
            for ev in b.r:
                self._wait(e, ev)


class Ring:
    def __init__(self, items):
        self.items = items
        self.i = 0

    def get(self):
        it = self.items[self.i % len(self.items)]
        self.i += 1
        return it


def build_nc(nst=NST, nexp=NEXP, nprev=NST):
    nc = bass.Bass("TRN2", target_bir_lowering=False)
    dt_in = lambda n, s: nc.dram_tensor(n, s, F32, kind="ExternalInput").ap()
    x_own = dt_in("x_own", [NTOK, D]); x_prev = dt_in("x_prev", [NTOK, D]); flag = dt_in("flag", [128, 1])
    c_col = dt_in("c_col", [128, 8]); ada_w = dt_in("ada_w", [D, 6 * D]); ada_b_bc = dt_in("ada_b_bc", [128, 6 * D])
    n1g_bc = dt_in("n1g_bc", [128, D]); n2g_bc = dt_in("n2g_bc", [128, D]); fg_bc = dt_in("fg_bc", [128, D])
    w_in = dt_in("w_in", [D, 1536]); w_out = dt_in("w_out", [D, D]); w_glu = dt_in("w_glu", [512, 512])
    router_w = dt_in("router_w", [D, 32]); router_b_bc = dt_in("router_b_bc", [128, 32])
    b_glu_col = dt_in("b_glu_col", [128, 4]); d_col = dt_in("d_col", [128, 4]); conv_b_col = dt_in("conv_b_col", [128, 4])
    ln_g_col = dt_in("ln_g_col", [128, 4]); ln_b_col = dt_in("ln_b_col", [128, 4]); og_col = dt_in("og_col", [128, 8])
    conv_wT = dt_in("conv_wT", [128, 4, 31])
    lamB_re = dt_in("lamB_re", [128, 4, 64]); lamB_im = dt_in("lamB_im", [128, 4, 64]); dtB = dt_in("dtB", [128, 4, 64])
    bB_re = dt_in("bB_re", [128, 4, 64]); bB_im = dt_in("bB_im", [128, 4, 64])
    cA_re = dt_in("cA_re", [128, 16, 16]); cA_im = dt_in("cA_im", [128, 16, 16])
    lamA_re = dt_in("lamA_re", [128, 16]); lamA_im = dt_in("lamA_im", [128, 16]); dtA = dt_in("dtA", [128, 16])
    maskB = dt_in("maskB", [128, 8])
    b_gate_r = dt_in("b_gate_r", [32, D]); b_up_r = dt_in("b_up_r", [32, D]); b_down = dt_in("b_down", [32, D])
    w_gate = dt_in("w_gate", [32, D, D]); w_up = dt_in("w_up", [32, D, D]); w_down = dt_in("w_down", [32, D, D])
    out = nc.dram_tensor("out", [NTOK, D], F32, kind="ExternalOutput").ap()
    thr_c = dt_in("thr_c", [128, NTOK // RS + 1]); ustrict = dt_in("ustrict", [128, 128]); jr_c = dt_in("jr_c", [128, NSLOT]); pcol_c = dt_in("pcol_c", [128, 3])
    x1s = nc.dram_tensor("x1s", [NTOK, D], F32, kind="Internal").ap()
    h2rows = nc.dram_tensor("h2rows", [NTOK, D], BF16, kind="Internal").ap()
    Xs = nc.dram_tensor("Xs", [NSLOT * RS, D], BF16, kind="Internal").ap()
    Ys = nc.dram_tensor("Ys", [NSLOT * RS, D], F32, kind="Internal").ap()
    wgb = nc.dram_tensor("wgb", [32, D, D], BF16, kind="Internal").ap()
    wub = nc.dram_tensor("wub", [32, D, D], BF16, kind="Internal").ap()
    wdb = nc.dram_tensor("wdb", [32, D, D], BF16, kind="Internal").ap()
    Bwconv = [[Buf(f"wcv{m}_{q}") for q in range(32)] for m in range(3)]
    Bcvkey = Buf("cvkey")
    Bwconv_all = Bwconv[0] + Bwconv[1] + Bwconv[2]
    Bx1s = [Buf(f"x1s{i}") for i in range(NST)]
    Bh2r = [Buf(f"h2r{i}") for i in range(NST * 4)]

    with ExitStack() as top:
        S = Sched(nc, top)

        def sbt(st, name, shape, dt):
            return st.enter_context(nc.sbuf_tensor(name, shape, dt)), Buf(name)

        def ring(st, name, shape, dt, n):
            return Ring([sbt(st, f"{name}{i}", shape, dt) for i in range(n)])

        banks = Ring([(top.enter_context(nc.psum_tensor(f"bank{i}", [128, 512], F32)), Buf(f"bank{i}")) for i in range(5)])
        cbank = (top.enter_context(nc.psum_tensor("cbank", [128, 512], F32)), Buf("cbank"))
        ybanks = Ring([(top.enter_context(nc.psum_tensor(f"ybank{i}", [128, 512], F32)), Buf(f"ybank{i}")) for i in range(2)])

        idf, Bidf = sbt(top, "idf", [128, 128], F32)
        idb, Bidb = sbt(top, "idb", [128, 128], BF16)
        onesb, Bones = sbt(top, "onesb", [128, 128], BF16)
        wts, Bwts = sbt(top, "wts", [128, 32, 32], F32)
        flg, Bflg = sbt(top, "flg", [128, 1], F32)
        g2b, Bg2b = sbt(top, "g2b", [128, D], BF16)
        lgs, Blgs = sbt(top, "lgs", [128, 32, 32], F32)
        mx4, Bmx4 = sbt(top, "mx4", [128, 32, 4], F32)
        w4, Bw4 = sbt(top, "w4", [128, 32, 4], F32)
        rank, Brank = sbt(top, "rank", [128, 32, 32], F32)
        cum, Bcum = sbt(top, "cum", [128, 1, 32], F32)
        ustb, Bustb = sbt(top, "ustb", [128, 128], BF16)
        S.dma("pool", ustb[:], ustrict, writes=[Bustb])
        S.op("pool", lambda e: e.memset(cum[:], 0.0), writes=[Bcum])
        S.op("pool", lambda e: e.memset(idf[:], 0.0), writes=[Bidf])
        S.op("pool", lambda e: e.affine_select(out=idf[:], in_=idf[:], pattern=[[-1, 128]], compare_op=ALU.not_equal,
                                               fill=1.0, base=0, channel_multiplier=1), reads=[Bidf], writes=[Bidf])
        S.op("dve", lambda e: e.tensor_copy(out=idb[:], in_=idf[:]), reads=[Bidf], writes=[Bidb])
        S.op("pool", lambda e: e.memset(onesb[:], 1.0), writes=[Bones])
        S.dma("sp", flg[:], flag, writes=[Bflg])
        conv_jobs = [(src_, dst_, Bwconv[m], ex) for ex in range(32) for m, (src_, dst_) in enumerate(((w_gate, wgb), (w_up, wub), (w_down, wdb)))]

        def issue_conv(n, pace=()):
            S._deps("pool", list(pace), [])
            for _ in range(n):
                if not conv_jobs:
                    return
                src_, dst_, Bd, ex = conv_jobs.pop(0)
                S.dma("pool", dst_[ex].rearrange("(a b) n -> a (b n)", a=16), src_[ex].rearrange("(a b) n -> a (b n)", a=16), writes=[Bd[ex]], key=Bcvkey)

        def transpose_to(dst_ap, Bdst, src_ap, Bsrc, ident, Bident, evac="act", nblk=1, dt=BF16):
            pst, Bpst = banks.get()
            pv = pst[:].bitcast(dt) if dt != F32 else pst[:]
            for j in range(nblk):
                S.op("pe", lambda e, j=j: e.transpose(out=pv[:, j * 128:(j + 1) * 128], in_=src_ap(j), identity=ident[:]),
                     reads=[Bsrc, Bident], writes=[Bpst])
            if evac == "act":
                S.op("act", lambda e: e.activation(out=dst_ap, in_=pv[:, 0:nblk * 128], func=AF.Identity), reads=[Bpst], writes=[Bdst])
            else:
                S.op(evac, lambda e: e.tensor_copy(out=dst_ap, in_=pv[:, 0:nblk * 128]), reads=[Bpst], writes=[Bdst])

        with ExitStack() as mx:
            mod, Bmod = sbt(mx, "mod", [128, 5 * D], BF16)
            winb, Bwin = sbt(mx, "winb", [128, 8, 1536], BF16)
            woutb, Bwout = sbt(mx, "woutb", [128, 8, D], BF16)
            wglub, Bwglu = sbt(mx, "wglub", [128, 4, 512], BF16)
            rwb, Brw = sbt(mx, "rwb", [128, 8, 32], BF16)
            rbb, Brbb = sbt(mx, "rbb", [128, 32], F32)
            cols, Bcols = sbt(mx, "cols", [128, 32], F32)
            cwt, Bcwt = sbt(mx, "cwt", [128, 4, 31], F32)
            BL, BBL = sbt(mx, "BL", [128, 16, 2, 128], BF16)
            CL, BCL = sbt(mx, "CL", [128, 16, 2, 128], BF16)
            Ec, BEc = sbt(mx, "Ec", [128, 16, TS], F32)
            Es, BEs = sbt(mx, "Es", [128, 16, TS], F32)
            rho, Brho = sbt(mx, "rho", [128, 16], F32)
            rho0, Brho0 = sbt(mx, "rho0", [128, 16, TS], F32)
            ddg, Bddg = sbt(mx, "ddg", [128, 4, 128], BF16)
            sre, Bsre = sbt(mx, "sre", [128, 16], F32)
            sim, Bsim = sbt(mx, "sim", [128, 16], F32)
            zbuf, Bzbuf = sbt(mx, "zbuf", [128, 4, 30 + 512], BF16)

            S.dma("sp", cols[:, 0:4], b_glu_col, writes=[Bcols]); S.dma("sp", cols[:, 4:8], d_col, writes=[Bcols])
            S.dma("sp", cols[:, 8:12], conv_b_col, writes=[Bcols]); S.dma("sp", cols[:, 12:16], ln_g_col, writes=[Bcols])
            S.dma("sp", cols[:, 16:20], ln_b_col, writes=[Bcols]); S.dma("sp", cols[:, 20:28], og_col, writes=[Bcols])
            S.dma("sp", rbb[:], router_b_bc, writes=[Brbb])
            S.dma("pool", winb[:], w_in.rearrange("(k p) n -> p k n", p=128), writes=[Bwin])
            S.dma("pool", woutb[:], w_out.rearrange("(k p) n -> p k n", p=128), writes=[Bwout])
            S.dma("pool", wglub[:], w_glu.rearrange("(k p) n -> p k n", p=128), writes=[Bwglu])
            S.dma("pool", rwb[:], router_w.rearrange("(k p) n -> p k n", p=128), writes=[Brw])
            Bsre_t = [Buf(f"sre_{t}") for t in range(4)]
            Bsim_t = [Buf(f"sim_{t}") for t in range(4)]
            S.op("pool", lambda e: e.memset(sre[:], 0.0), writes=[Bsre] + Bsre_t)
            S.op("pool", lambda e: e.memset(sim[:], 0.0), writes=[Bsim] + Bsim_t)
            S.op("pool", lambda e: e.memset(zbuf[:], 0.0), writes=[Bzbuf])

            with ExitStack() as su:
                ccol, Bccol = sbt(su, "ccol", [128, 8], F32)
                crep, Bcrep = sbt(su, "crep", [128, 8, 128], F32)
                awr = ring(su, "aw", [128, 8, 512], F32, 2)
                abr = ring(su, "ab", [128, 512], F32, 2)
                S.dma("sp", ccol[:], c_col, writes=[Bccol])
                S.op("act", lambda e: e.activation(out=ccol[:], in_=ccol[:], func=AF.Silu), reads=[Bccol], writes=[Bccol])
                for k in range(8):
                    S.op("dve", lambda e, k=k: e.tensor_copy(out=crep[:, k, :], in_=ccol[:, k:k + 1].to_broadcast([128, 128])),
                         reads=[Bccol], writes=[Bcrep])
                awv = ada_w.rearrange("(k p) n -> p k n", p=128)
                for nt in range(12):
                    aw, Baw = awr.get(); ab, Bab = abr.get()
                    S.dma("sp", aw[:], awv[:, :, nt * 512:(nt + 1) * 512], writes=[Baw])
                    S.dma("act", ab[:], ada_b_bc[:, nt * 512:(nt + 1) * 512], writes=[Bab])
                    ps, Bps = banks.get()
                    for k in range(8):
                        S.op("pe", lambda e, k=k: e.matmul(ps[:], lhsT=crep[:, k, :], rhs=aw[:, k, :], start=(k == 0), stop=(k == 7)),
                             reads=[Bcrep, Baw], writes=[Bps])
                    if nt < 10:
                        S.op("dve", lambda e: e.tensor_tensor(out=mod[:, nt * 512:(nt + 1) * 512], in0=ps[:], in1=ab[:], op=ALU.add),
                             reads=[Bps, Bab], writes=[Bmod])
                    else:
                        S.op("dve", lambda e: e.tensor_tensor(out=g2b[:, (nt - 10) * 512:(nt - 9) * 512], in0=ps[:], in1=ab[:], op=ALU.add),
                             reads=[Bps, Bab], writes=[Bg2b])
                gt_, Bgt = sbt(su, "gtmp", [128, D], F32)
                for (gsrc, lo) in ((n1g_bc, 1024), (n2g_bc, 4096)):
                    S.dma("sp", gt_[:], gsrc, writes=[Bgt])
                    S.op("dve", lambda e, lo=lo: e.scalar_tensor_tensor(out=mod[:, lo:lo + D], in0=mod[:, lo:lo + D], scalar=1.0,
                                                                      in1=gt_[:], op0=ALU.add, op1=ALU.mult),
                         reads=[Bmod, Bgt], writes=[Bmod])
                S.dma("sp", cwt[:], conv_wT, writes=[Bcwt])

                def cossin(tag, th, Bth, shape, n):
                    y, By = sbt(su, tag + "y", shape, F32); ki, Bki = sbt(su, tag + "ki", shape, I32)
                    f, Bf = sbt(su, tag + "f", shape, F32); m, Bm = sbt(su, tag + "m", shape, F32)
                    g, Bg = sbt(su, tag + "g", shape, F32)
                    co, Bco = sbt(su, tag + "co", shape, F32); si, Bsi = sbt(su, tag + "si", shape, F32)
                    S.op("dve", lambda e: e.tensor_scalar(out=y[:], in0=th[:], scalar1=1.0 / TWO_PI, scalar2=None, op0=ALU.mult), reads=[Bth], writes=[By])
                    S.op("dve", lambda e: e.tensor_copy(out=ki[:], in_=y[:]), reads=[By], writes=[Bki])
                    S.op("dve", lambda e: e.tensor_copy(out=f[:], in_=ki[:]), reads=[Bki], writes=[Bf])
                    S.op("dve", lambda e: e.tensor_tensor(out=f[:], in0=y[:], in1=f[:], op=ALU.subtract), reads=[By, Bf], writes=[Bf])

                    def wrap(t, Bt):
                        S.op("dve", lambda e: e.tensor_scalar(out=m[:], in0=t[:], scalar1=0.5, scalar2=None, op0=ALU.is_gt), reads=[Bt], writes=[Bm])
                        S.op("dve", lambda e: e.tensor_tensor(out=t[:], in0=t[:], in1=m[:], op=ALU.subtract), reads=[Bt, Bm], writes=[Bt])
                        S.op("dve", lambda e: e.tensor_scalar(out=m[:], in0=t[:], scalar1=-0.5, scalar2=None, op0=ALU.is_lt), reads=[Bt], writes=[Bm])
                        S.op("dve", lambda e: e.tensor_tensor(out=t[:], in0=t[:], in1=m[:], op=ALU.add), reads=[Bt, Bm], writes=[Bt])
                    wrap(f, Bf)
                    S.op("dve", lambda e: e.tensor_scalar(out=g[:], in0=f[:], scalar1=0.25, scalar2=None, op0=ALU.add), reads=[Bf], writes=[Bg])
                    wrap(g, Bg)
                    S.op("act", lambda e: e.activation(out=si[:], in_=f[:], func=AF.Sin, scale=TWO_PI), reads=[Bf], writes=[Bsi])
                    S.op("act", lambda e: e.activation(out=co[:], in_=g[:], func=AF.Sin, scale=TWO_PI), reads=[Bg], writes=[Bco])
                    return (co, Bco), (si, Bsi)

                def abar(tag, lre_d, lim_d, dt_d, shape):
                    lr, Blr = sbt(su, tag + "lr", shape, F32); li, Bli = sbt(su, tag + "li", shape, F32)
                    dv, Bdv = sbt(su, tag + "dv", shape, F32); mg, Bmg = sbt(su, tag + "mg", shape, F32)
                    th, Bth = sbt(su, tag + "th", shape, F32)
                    ar, Bar = sbt(su, tag + "ar", shape, F32); ai, Bai = sbt(su, tag + "ai", shape, F32)
                    S.dma("sp", lr[:], lre_d, writes=[Blr]); S.dma("sp", li[:], lim_d, writes=[Bli]); S.dma("sp", dv[:], dt_d, writes=[Bdv])
                    S.op("act", lambda e: e.activation(out=dv[:], in_=dv[:], func=AF.Exp), reads=[Bdv], writes=[Bdv])
                    S.op("dve", lambda e: e.tensor_tensor(out=mg[:], in0=lr[:], in1=dv[:], op=ALU.mult), reads=[Blr, Bdv], writes=[Bmg])
                    S.op("act", lambda e: e.activation(out=mg[:], in_=mg[:], func=AF.Exp), reads=[Bmg], writes=[Bmg])
                    S.op("dve", lambda e: e.tensor_tensor(out=th[:], in0=li[:], in1=dv[:], op=ALU.mult), reads=[Bli, Bdv], writes=[Bth])
                    (co, Bco), (si, Bsi) = cossin(tag, th, Bth, shape, 0)
                    S.op("dve", lambda e: e.tensor_tensor(out=ar[:], in0=mg[:], in1=co[:], op=ALU.mult), reads=[Bmg, Bco], writes=[Bar])
                    S.op("dve", lambda e: e.tensor_tensor(out=ai[:], in0=mg[:], in1=si[:], op=ALU.mult), reads=[Bmg, Bsi], writes=[Bai])
                    return (ar, Bar), (ai, Bai), (lr, Blr), (li, Bli), (mg, Bmg), (co, Bco), (si, Bsi)

                shB = [128, 4, 64]
                (ar, Bar), (ai, Bai), (lr, Blr), (li, Bli), _, _, _ = abar("B", lamB_re, lamB_im, dtB, shB)
                den, Bden = sbt(su, "den", shB, F32); t1, Bt1 = sbt(su, "t1", shB, F32); t2, Bt2 = sbt(su, "t2", shB, F32)
                qr, Bqr = sbt(su, "qr", shB, F32); qi, Bqi = sbt(su, "qi", shB, F32)
                br_, Bbr = sbt(su, "br_", shB, F32); bi_, Bbi = sbt(su, "bi_", shB, F32)
                bbr, Bbbr = sbt(su, "bbr", shB, F32); bbi, Bbbi = sbt(su, "bbi", shB, F32)
                tt = lambda o, Bo, a, Ba, b, Bb, op: S.op("dve", lambda e: e.tensor_tensor(out=o[:], in0=a[:], in1=b[:], op=op), reads=[Ba, Bb], writes=[Bo])
                tt(den, Bden, lr, Blr, lr, Blr, ALU.mult); tt(t1, Bt1, li, Bli, li, Bli, ALU.mult); tt(den, Bden, den, Bden, t1, Bt1, ALU.add)
                S.op("dve", lambda e: e.reciprocal(out=den[:], in_=den[:]), reads=[Bden], writes=[Bden])
                S.op("dve", lambda e: e.tensor_scalar(out=ar[:], in0=ar[:], scalar1=-1.0, scalar2=None, op0=ALU.add), reads=[Bar], writes=[Bar])
                tt(t1, Bt1, ar, Bar, lr, Blr, ALU.mult); tt(t2, Bt2, ai, Bai, li, Bli, ALU.mult); tt(qr, Bqr, t1, Bt1, t2, Bt2, ALU.add)
                tt(qr, Bqr, qr, Bqr, den, Bden, ALU.mult)
                tt(t1, Bt1, ai, Bai, lr, Blr, ALU.mult); tt(t2, Bt2, ar, Bar, li, Bli, ALU.mult); tt(qi, Bqi, t1, Bt1, t2, Bt2, ALU.subtract)
                tt(qi, Bqi, qi, Bqi, den, Bden, ALU.mult)
                S.dma("sp", br_[:], bB_re, writes=[Bbr]); S.dma("sp", bi_[:], bB_im, writes=[Bbi])
                tt(t1, Bt1, qr, Bqr, br_, Bbr, ALU.mult); tt(t2, Bt2, qi, Bqi, bi_, Bbi, ALU.mult); tt(bbr, Bbbr, t1, Bt1, t2, Bt2, ALU.subtract)
                tt(t1, Bt1, qr, Bqr, bi_, Bbi, ALU.mult); tt(t2, Bt2, qi, Bqi, br_, Bbr, ALU.mult); tt(bbi, Bbbi, t1, Bt1, t2, Bt2, ALU.add)
                mkb, Bmkb = sbt(su, "mkb", [128, 8], F32)
                S.dma("sp", mkb[:], maskB, writes=[Bmkb])
                for pair in range(16):
                    t, pl = pair // 4, pair % 4
                    for ri, (src, Bsrc) in enumerate(((bbr, Bbbr), (bbi, Bbbi))):
                        for half in range(2):
                            S.op("dve", lambda e, pair=pair, ri=ri, half=half, src=src, t=t, pl=pl: e.tensor_scalar(
                                out=BL[:, pair, ri, half * 64:(half + 1) * 64], in0=src[:, t, :],
                                scalar1=mkb[:, 2 * pl + half:2 * pl + half + 1], scalar2=None, op0=ALU.mult),
                                reads=[Bsrc, Bmkb], writes=[BBL])
                car, Bcar = sbt(su, "car", [128, 16, 16], F32); cai, Bcai = sbt(su, "cai", [128, 16, 16], F32)
                S.dma("sp", car[:], cA_re, writes=[Bcar]); S.dma("sp", cai[:], cA_im, writes=[Bcai])
                S.op("pool", lambda e: e.memset(CL[:], 0.0), writes=[BCL])
                for pair in range(16):
                    pl = pair % 4
                    for half in range(2):
                        c0 = 32 * pl + 16 * half
                        S.op("dve", lambda e, pair=pair, half=half, c0=c0: e.tensor_copy(
                            out=CL[half * 64:(half + 1) * 64, pair, 0, c0:c0 + 16], in_=car[half * 64:(half + 1) * 64, pair, :]),
                            reads=[Bcar], writes=[BCL])
                        S.op("dve", lambda e, pair=pair, half=half, c0=c0: e.tensor_scalar(
                            out=CL[half * 64:(half + 1) * 64, pair, 1, c0:c0 + 16], in0=cai[half * 64:(half + 1) * 64, pair, :],
                            scalar1=-1.0, scalar2=None, op0=ALU.mult), reads=[Bcai], writes=[BCL])
                shA = [128, 16]
                _, _, _, _, (mgA, BmgA), (coA, BcoA), (siA, BsiA) = abar("A", lamA_re, lamA_im, dtA, shA)
                S.op("dve", lambda e: e.tensor_copy(out=rho[:], in_=mgA[:]), reads=[BmgA], writes=[Brho])
                S.op("dve", lambda e: e.tensor_copy(out=rho0[:], in_=mgA[:].rearrange("p (a o) -> p a o", o=1).to_broadcast([128, 16, TS])), reads=[BmgA], writes=[Brho0])
                S.op("dve", lambda e: e.memset(rho0[:, :, 0:1], 0.0), reads=[], writes=[Brho0])
                for t in range(4):
                    S.op("dve", lambda e: e.tensor_scalar(out=ddg[:, t, :], in0=idf[:], scalar1=cols[:, 4 + t:5 + t], scalar2=None, op0=ALU.mult), reads=[Bidf, Bcols], writes=[Bddg])
                S.op("dve", lambda e: e.tensor_copy(out=Ec[:, :, 0], in_=coA[:]), reads=[BcoA], writes=[BEc])
                S.op("dve", lambda e: e.tensor_copy(out=Es[:, :, 0], in_=siA[:]), reads=[BsiA], writes=[BEs])
                ta, Bta = sbt(su, "ta", [128, 16, TS // 2], F32); tb, Btb = sbt(su, "tb", [128, 16, TS // 2], F32)
                m = 1
                while m < TS:
                    cm = Ec[:, :, m - 1:m].to_broadcast([128, 16, m]); sm = Es[:, :, m - 1:m].to_broadcast([128, 16, m])
                    S.op("dve", lambda e, m=m, cm=cm: e.tensor_tensor(out=ta[:, :, 0:m], in0=Ec[:, :, 0:m], in1=cm, op=ALU.mult), reads=[BEc], writes=[Bta])
                    S.op("dve", lambda e, m=m, sm=sm: e.tensor_tensor(out=tb[:, :, 0:m], in0=Es[:, :, 0:m], in1=sm, op=ALU.mult), reads=[BEs], writes=[Btb])
                    S.op("dve", lambda e, m=m: e.tensor_tensor(out=ta[:, :, 0:m], in0=ta[:, :, 0:m], in1=tb[:, :, 0:m], op=ALU.subtract), reads=[Bta, Btb], writes=[Bta])
                    S.op("dve", lambda e, m=m, sm=sm: e.tensor_tensor(out=tb[:, :, 0:m], in0=Ec[:, :, 0:m], in1=sm, op=ALU.mult), reads=[BEc, BEs], writes=[Btb])
                    S.op("dve", lambda e, m=m: e.tensor_copy(out=Ec[:, :, m:2 * m], in_=ta[:, :, 0:m]), reads=[Bta], writes=[BEc])
                    S.op("dve", lambda e, m=m, cm=cm: e.tensor_tensor(out=ta[:, :, 0:m], in0=Es[:, :, 0:m], in1=cm, op=ALU.mult), reads=[BEs, BEc], writes=[Bta])
                    S.op("dve", lambda e, m=m: e.tensor_tensor(out=Es[:, :, m:2 * m], in0=ta[:, :, 0:m], in1=tb[:, :, 0:m], op=ALU.add), reads=[Bta, Btb], writes=[BEs])
                    m *= 2

            S.barrier()
            with ExitStack() as ml:
                xr = ring(ml, "xt", [128, D], F32, 2)
                hr = ring(ml, "ht", [128, D], BF16, 1)
                st4 = ring(ml, "st4", [128, 8], F32, 4)
                hT, BhT = sbt(ml, "hT", [128, 8, 512], BF16)
                uTb, BuTb = sbt(ml, "uTb", [128, 4, 512], BF16)
                sg, Bsg = sbt(ml, "sg", [128, 512], BF16)
                Abig = [sbt(ml, f"Abig{i}", [128, 2048], F32) for i in range(4)]
                xbr = ring(ml, "xb", [128, 512], BF16, 2)
                st8, Bst8 = sbt(ml, "st8", [128, 64], F32)
                dgr = ring(ml, "dg", [128, 128], BF16, 4)
                cv, Bcv = sbt(ml, "cv", [128, 4, 512], BF16)
                conv_state = {"todo": []}

                def conv_fill(n):
                    cps, Bcps = cbank
                    for _ in range(n):
                        if not conv_state["todo"]:
                            return
                        c, j = conv_state["todo"].pop(0)
                        dgt, Bdgt = dgr.get()
                        S.op("dve", lambda e: e.tensor_scalar(out=dgt[:], in0=idf[:], scalar1=cwt[:, c, j:j + 1], scalar2=None, op0=ALU.mult), reads=[Bidf, Bcwt], writes=[Bdgt])
                        S.op("pe", lambda e: e.matmul(cps[:], lhsT=dgt[:], rhs=zbuf[:, c, j:j + 512], start=(j == 0), stop=(j == 30)), reads=[Bdgt, Bzbuf], writes=[Bcps])
                        if j == 30:
                            S.op("act", lambda e: e.activation(out=cv[:, c, :], in_=cps[:], func=AF.Identity, bias=cols[:, 8 + c:9 + c]), reads=[Bcps, Bcols], writes=[Bcv])
                Bst8_t = [Buf(f"st8_{t}") for t in range(4)]
                mg_, Bmg_ = sbt(ml, "mg", [128, 8, 512], BF16)
                q, Bq = sbt(ml, "q", [128, 4, 512], F32)
                ys, Bys = q, Bq
                sq, Bsq = q[:, 0:2, :].rearrange("p a b -> p (a b)"), Bq
                Bqc = [Buf(f"qc{c}") for c in range(4)]
                qb, Bqb = sbt(ml, "qb", [128, 4, 512], BF16)
                q2, Bq2 = qb, Bqb
                yg, Byg = qb, Bqb
                nr_ = ring(ml, "nr", [128, 512], F32, 2)
                x1r = ring(ml, "x1t", [128, D], F32, 1)
                h2T, Bh2T = hT, BhT
                lg, Blg = sbt(ml, "lg", [128, 32], F32)
                mx8, Bmx8 = sbt(ml, "mx8", [128, 8], F32)
                msk, Bmsk = sbt(ml, "msk", [128, 32], F32)
                mskb, Bmskb = sbt(ml, "mskb", [128, 32], BF16)
                sm4, Bsm4 = sbt(ml, "sm4", [128, 4], F32)

                def rstd_of(xt, Bxt, col, Bcol, n=D):
                    S.op("act", lambda e: e.activation(out=sq[:, 0:n], in_=xt, func=AF.Square, accum_out=col[:, 1:2]), reads=[Bxt], writes=[Bsq, Bcol])
                    S.op("dve", lambda e: e.tensor_scalar(out=col[:, 2:3], in0=col[:, 1:2], scalar1=1.0 / n, scalar2=1e-6, op0=ALU.mult, op1=ALU.add), reads=[Bcol], writes=[Bcol])
                    S.op("act", lambda e: e.activation(out=col[:, 3:4], in_=col[:, 2:3], func=AF.Sqrt), reads=[Bcol], writes=[Bcol])
                    S.op("dve", lambda e: e.reciprocal(out=col[:, 0:1], in_=col[:, 3:4]), reads=[Bcol], writes=[Bcol])

                def norm_mod_T(xt, Bxt, a_lo, sh_lo, dstT, BdstT, tcol, store=None):
                    col, Bc = st4.get()
                    rstd_of(xt[:], Bxt, col, Bc)
                    h, Bh = hr.get()
                    S.op("dve", lambda e: e.scalar_tensor_tensor(out=sq[:], in0=xt[:], scalar=col[:, 0:1], in1=mod[:, a_lo:a_lo + D], op0=ALU.mult, op1=ALU.mult),
                         reads=[Bxt, Bc, Bmod], writes=[Bsq])
                    S.op("dve", lambda e: e.tensor_tensor(out=h[:], in0=sq[:], in1=mod[:, sh_lo:sh_lo + D], op=ALU.add), reads=[Bsq, Bmod], writes=[Bh])
                    if store is not None:
                        S.dma("sp", h2rows[store * 128:(store + 1) * 128, :], h[:], reads=[Bh], writes=[Bh2r[store]], key=Bh)
                    pst, Bpst = banks.get()
                    pv = pst[:].bitcast(BF16)
                    for k in range(8):
                        S.op("pe", lambda e, k=k: e.transpose(out=pv[:, k * 128:(k + 1) * 128], in_=h[:, k * 128:(k + 1) * 128], identity=idb[:]),
                             reads=[Bh, Bidb], writes=[Bpst])
                    S.op("act", lambda e: e.activation(out=dstT[:, :, tcol * 128:(tcol + 1) * 128], in_=pv[:, 0:1024].rearrange("p (k t) -> p k t", k=8), func=AF.Identity),
                         reads=[Bpst], writes=[BdstT])

                def bsum_bc(src, Bsrc, nch, c0=0):
                    ps, Bps = banks.get()
                    for k in range(nch):
                        S.op("pe", lambda e, k=k: e.matmul(ps[:], lhsT=onesb[:], rhs=src[:, c0 + k, :], start=(k == 0), stop=(k == nch - 1)),
                             reads=[Bones, Bsrc], writes=[Bps])
                    return ps, Bps

                def rstd_bc(ps, Bps, n, eps):
                    r, Br = nr_.get()
                    S.op("dve", lambda e: e.tensor_scalar(out=r[:], in0=ps[:], scalar1=1.0 / n, scalar2=eps, op0=ALU.mult, op1=ALU.add), reads=[Bps], writes=[Br])
                    S.op("act", lambda e: e.activation(out=r[:], in_=r[:], func=AF.Sqrt), reads=[Br], writes=[Br])
                    S.op("dve", lambda e: e.reciprocal(out=r[:], in_=r[:]), reads=[Br], writes=[Br])
                    return r, Br

                def ssm(own):
                    NSC = 512 // TS
                    EcA = Ec[:].rearrange("p a b -> p (a b)"); EsA = Es[:].rearrange("p a b -> p (a b)"); R0A = rho0[:].rearrange("p a b -> p (a b)")
                    (A1, BA1), (A2, BA2), (A3, BA3), (A4, BA4) = Abig
                    A1v = A1[:].rearrange("p (a b) -> p a b", a=16); A3v = A3[:].rearrange("p (a b) -> p a b", a=16)
                    A2v = A2[:].rearrange("p (a b) -> p a b", a=16); A4v = A4[:].rearrange("p (a b) -> p a b", a=16)
                    for sc in range(NSC):
                        c0 = sc * TS
                        if own:
                            psy, Bpsy = ybanks.get()
                        for t in range(4):
                            p0 = 4 * t
                            sl_ = slice(t * 512, (t + 1) * 512)
                            psr, Bpsr = banks.get(); psi, Bpsi = banks.get()
                            for pl in range(4):
                                S.op("pe", lambda e: e.matmul(psr[:, pl * TS:(pl + 1) * TS], lhsT=BL[:, p0 + pl, 0, :], rhs=uTb[:, t, c0:c0 + TS], start=True, stop=True), reads=[BBL, BuTb], writes=[Bpsr])
                            for pl in range(4):
                                S.op("pe", lambda e: e.matmul(psi[:, pl * TS:(pl + 1) * TS], lhsT=BL[:, p0 + pl, 1, :], rhs=uTb[:, t, c0:c0 + TS], start=True, stop=True), reads=[BBL, BuTb], writes=[Bpsi])
                            S.op("dve", lambda e: e.tensor_tensor(out=A1[:, sl_], in0=psr[:], in1=EcA[:, sl_], op=ALU.mult), reads=[Bpsr, BEc], writes=[BA1])
                            S.op("dve", lambda e: e.tensor_tensor(out=A2[:, sl_], in0=psi[:], in1=EsA[:, sl_], op=ALU.mult), reads=[Bpsi, BEs], writes=[BA2])
                            S.op("dve", lambda e: e.tensor_tensor(out=A3[:, sl_], in0=psi[:], in1=EcA[:, sl_], op=ALU.mult), reads=[Bpsi, BEc], writes=[BA3])
                            S.op("dve", lambda e: e.tensor_tensor(out=A4[:, sl_], in0=psr[:], in1=EsA[:, sl_], op=ALU.mult), reads=[Bpsr, BEs], writes=[BA4])
                            if own:
                                conv_fill(8)
                        S.op("dve", lambda e: e.tensor_tensor(out=A1[:], in0=A1[:], in1=A2[:], op=ALU.add), reads=[BA1, BA2], writes=[BA1])
                        S.op("dve", lambda e: e.tensor_tensor(out=A3[:], in0=A3[:], in1=A4[:], op=ALU.subtract), reads=[BA3, BA4], writes=[BA3])
                        S.op("dve", lambda e: e.tensor_tensor(out=st8[:, 0:16], in0=rho[:], in1=sre[:], op=ALU.mult), reads=[Brho, Bsre], writes=[Bst8])
                        S.op("dve", lambda e: e.tensor_tensor(out=st8[:, 16:32], in0=rho[:], in1=sim[:], op=ALU.mult), reads=[Brho, Bsim], writes=[Bst8])
                        S.op("dve", lambda e: e.tensor_tensor(out=A1v[:, :, 0], in0=A1v[:, :, 0], in1=st8[:, 0:16], op=ALU.add), reads=[BA1, Bst8], writes=[BA1])
                        S.op("dve", lambda e: e.tensor_tensor(out=A3v[:, :, 0], in0=A3v[:, :, 0], in1=st8[:, 16:32], op=ALU.add), reads=[BA3, Bst8], writes=[BA3])
                        if own:
                            conv_fill(16)
                        for t in range(4):
                            sl_ = slice(t * 512, (t + 1) * 512)
                            S.op("dve", lambda e: e.tensor_tensor_scan(out=A2[:, sl_], data0=R0A[:, sl_], data1=A1[:, sl_], initial=0.0, op0=ALU.mult, op1=ALU.add), reads=[Brho0, BA1], writes=[BA2])
                            S.op("dve", lambda e: e.tensor_tensor_scan(out=A4[:, sl_], data0=R0A[:, sl_], data1=A3[:, sl_], initial=0.0, op0=ALU.mult, op1=ALU.add), reads=[Brho0, BA3], writes=[BA4])
                        if own:
                            S.op("dve", lambda e: e.tensor_tensor(out=A1[:], in0=A2[:], in1=EcA, op=ALU.mult), reads=[BA2, BEc], writes=[BA1])
                            S.op("dve", lambda e: e.tensor_tensor(out=A3[:], in0=A4[:], in1=EsA, op=ALU.mult), reads=[BA4, BEs], writes=[BA3])
                            S.op("dve", lambda e: e.tensor_tensor(out=A2[:], in0=A2[:], in1=EsA, op=ALU.mult), reads=[BA2, BEs], writes=[BA2])
                            S.op("dve", lambda e: e.tensor_tensor(out=A4[:], in0=A4[:], in1=EcA, op=ALU.mult), reads=[BA4, BEc], writes=[BA4])
                            S.op("dve", lambda e: e.tensor_tensor(out=A1[:], in0=A1[:], in1=A3[:], op=ALU.subtract), reads=[BA1, BA3], writes=[BA1])
                            S.op("dve", lambda e: e.tensor_tensor(out=A4[:], in0=A4[:], in1=A2[:], op=ALU.add), reads=[BA4, BA2], writes=[BA4])
                            S.op("dve", lambda e: e.tensor_copy(out=sre[:], in_=A1v[:, :, TS - 1]), reads=[BA1], writes=[Bsre])
                            S.op("dve", lambda e: e.tensor_copy(out=sim[:], in_=A4v[:, :, TS - 1]), reads=[BA4], writes=[Bsim])
                            for t in range(4):
                                p0 = 4 * t
                                sl_ = slice(t * 512, (t + 1) * 512)
                                (xb1, Bxb1), (xb2, Bxb2) = xbr.get(), xbr.get()
                                S.op("act", lambda e: e.activation(out=xb1[:], in_=A1[:, sl_], func=AF.Identity), reads=[BA1], writes=[Bxb1])
                                S.op("act", lambda e: e.activation(out=xb2[:], in_=A4[:, sl_], func=AF.Identity), reads=[BA4], writes=[Bxb2])
                                for pl in range(4):
                                    S.op("pe", lambda e: e.matmul(psy[:, t * TS:(t + 1) * TS], lhsT=CL[:, p0 + pl, 0, :], rhs=xb1[:, pl * TS:(pl + 1) * TS], start=(pl == 0), stop=False), reads=[BCL, Bxb1], writes=[Bpsy])
                                    S.op("pe", lambda e: e.matmul(psy[:, t * TS:(t + 1) * TS], lhsT=CL[:, p0 + pl, 1, :], rhs=xb2[:, pl * TS:(pl + 1) * TS], start=False, stop=False), reads=[BCL, Bxb2], writes=[Bpsy])
                                S.op("pe", lambda e: e.matmul(psy[:, t * TS:(t + 1) * TS], lhsT=ddg[:, t, :], rhs=uTb[:, t, c0:c0 + TS], start=False, stop=True), reads=[Bddg, BuTb], writes=[Bpsy])
                            S.op("act", lambda e: e.activation(out=ys[:, :, c0:c0 + TS], in_=psy[:].rearrange("p (a b) -> p a b", a=4), func=AF.Identity), reads=[Bpsy], writes=[Bys])
                        else:
                            EcL = Ec[:, :, TS - 1]; EsL = Es[:, :, TS - 1]
                            v1L = A2v[:, :, TS - 1]; v2L = A4v[:, :, TS - 1]
                            S.op("dve", lambda e: e.tensor_tensor(out=st8[:, 32:48], in0=v1L, in1=EcL, op=ALU.mult), reads=[BA2, BEc], writes=[Bst8])
                            S.op("dve", lambda e: e.tensor_tensor(out=st8[:, 48:64], in0=v2L, in1=EsL, op=ALU.mult), reads=[BA4, BEs], writes=[Bst8])
                            S.op("dve", lambda e: e.tensor_tensor(out=sre[:], in0=st8[:, 32:48], in1=st8[:, 48:64], op=ALU.subtract), reads=[Bst8], writes=[Bsre])
                            S.op("dve", lambda e: e.tensor_tensor(out=st8[:, 32:48], in0=v2L, in1=EcL, op=ALU.mult), reads=[BA4, BEc], writes=[Bst8])
                            S.op("dve", lambda e: e.tensor_tensor(out=st8[:, 48:64], in0=v1L, in1=EsL, op=ALU.mult), reads=[BA2, BEs], writes=[Bst8])
                            S.op("dve", lambda e: e.tensor_tensor(out=sim[:], in0=st8[:, 32:48], in1=st8[:, 48:64], op=ALU.add), reads=[Bst8], writes=[Bsim])

                def inproj(chunks):
                    for oc in chunks:
                        ps, Bps = banks.get()
                        for k in range(8):
                            S.op("pe", lambda e, k=k: e.matmul(ps[:], lhsT=winb[:, k, oc * 128:(oc + 1) * 128], rhs=hT[:, k, :], start=(k == 0), stop=(k == 7)),
                                 reads=[Bwin, BhT], writes=[Bps])
                        yield oc, ps, Bps

                def front(xsrc, sti, own, need_z):
                    xts = []
                    for tl in range(4):
                        xt, Bxt = xr.get()
                        r0 = sti * 512 + tl * 128
                        S.dma("sp", xt[:], xsrc[r0:r0 + 128, :], writes=[Bxt])
                        norm_mod_T(xt, Bxt, 1024, 0, hT, BhT, tl)
                        xts.append((xt, Bxt))
                    for oc, ps, Bps in inproj(range(4)):
                        S.op("act", lambda e: e.activation(out=uTb[:, oc, :], in_=ps[:], func=AF.Identity), reads=[Bps], writes=[BuTb])
                    if need_z:
                        for c in range(4):
                            gen = inproj([8 + c, 4 + c])
                            _, psg, Bpsg = next(gen)
                            S.op("act", lambda e: e.activation(out=sg[:], in_=psg[:], func=AF.Sigmoid), reads=[Bpsg], writes=[Bsg])
                            _, psv, Bpsv = next(gen)
                            S.op("dve", lambda e: e.tensor_tensor(out=zbuf[:, c, 30:542], in0=psv[:], in1=sg[:], op=ALU.mult), reads=[Bpsv, Bsg], writes=[Bzbuf])
                    return xts

                for sti in range(NST - nprev, NST):
                    last = (sti == NST - 1)
                    issue_conv(6, pace=[BuTb])
                    front(x_prev, sti, False, last)
                    ssm(False)
                    if last:
                        S.op("dve", lambda e: e.tensor_scalar(out=zbuf[:, :, 0:30], in0=zbuf[:, :, 512:542], scalar1=flg[:, 0:1], scalar2=None, op0=ALU.mult),
                             reads=[Bzbuf, Bflg], writes=[Bzbuf])
                S.op("dve", lambda e: e.tensor_scalar(out=sre[:], in0=sre[:], scalar1=flg[:, 0:1], scalar2=None, op0=ALU.mult), reads=[Bsre, Bflg], writes=[Bsre])
                S.op("dve", lambda e: e.tensor_scalar(out=sim[:], in0=sim[:], scalar1=flg[:, 0:1], scalar2=None, op0=ALU.mult), reads=[Bsim, Bflg], writes=[Bsim])

                for sti in range(nst):
                    issue_conv(6, pace=[BuTb])
                    xts = front(x_own, sti, True, True)
                    conv_state["todo"] = [(c, j) for c in range(4) for j in range(31)]
                    ssm(True)
                    conv_fill(200)
                    S.op("act", lambda e: e.activation(out=yg[:], in_=ys[:], func=AF.Gelu), reads=[Bys], writes=[Byg])
                    for oc in range(4):
                        ps, Bps = banks.get()
                        for k in range(4):
                            S.op("pe", lambda e, k=k: e.matmul(ps[:], lhsT=wglub[:, k, oc * 128:(oc + 1) * 128], rhs=yg[:, k, :], start=(k == 0), stop=(k == 3)),
                                 reads=[Bwglu, Byg], writes=[Bps])
                        S.op("act", lambda e: e.activation(out=sg[:], in_=ps[:], func=AF.Sigmoid, bias=cols[:, oc:oc + 1]), reads=[Bps, Bcols], writes=[Bsg])
                        S.op("dve", lambda e: e.tensor_tensor(out=q[:, oc, :], in0=yg[:, oc, :], in1=sg[:], op=ALU.mult), reads=[Byg, Bsg], writes=[Bq])
                    S.op("act", lambda e: e.activation(out=q2[:], in_=q[:], func=AF.Square), reads=[Bq], writes=[Bq2])
                    ps, Bps = bsum_bc(q2, Bq2, 4)
                    r, Br = rstd_bc(ps, Bps, 512, 1e-6)
                    for oc in range(4):
                        S.op("dve", lambda e: e.scalar_tensor_tensor(out=mg_[:, oc, :], in0=q[:, oc, :], scalar=cols[:, 20 + oc:21 + oc], in1=r[:], op0=ALU.mult, op1=ALU.mult),
                             reads=[Bq, Bcols, Br], writes=[Bmg_])
                    S.op("dve", lambda e: e.tensor_copy(out=zbuf[:, :, 0:30], in_=zbuf[:, :, 512:542]), reads=[Bzbuf], writes=[Bzbuf])
                    ps1, Bps1 = bsum_bc(cv, Bcv, 4)
                    S.op("act", lambda e: e.activation(out=q2[:], in_=cv[:], func=AF.Square), reads=[Bcv], writes=[Bq2])
                    ps2, Bps2 = bsum_bc(q2, Bq2, 4)
                    mu, Bmu = nr_.get(); var, Bvar = nr_.get()
                    S.op("dve", lambda e: e.tensor_scalar(out=mu[:], in0=ps1[:], scalar1=1.0 / 512, scalar2=None, op0=ALU.mult), reads=[Bps1], writes=[Bmu])
                    S.op("dve", lambda e: e.tensor_tensor(out=var[:], in0=mu[:], in1=mu[:], op=ALU.mult), reads=[Bmu], writes=[Bvar])
                    S.op("dve", lambda e: e.scalar_tensor_tensor(out=var[:], in0=ps2[:], scalar=1.0 / 512, in1=var[:], op0=ALU.mult, op1=ALU.subtract), reads=[Bps2, Bvar], writes=[Bvar])
                    S.op("dve", lambda e: e.tensor_scalar(out=var[:], in0=var[:], scalar1=1e-5, scalar2=None, op0=ALU.add), reads=[Bvar], writes=[Bvar])
                    S.op("act", lambda e: e.activation(out=var[:], in_=var[:], func=AF.Sqrt), reads=[Bvar], writes=[Bvar])
                    S.op("dve", lambda e: e.reciprocal(out=var[:], in_=var[:]), reads=[Bvar], writes=[Bvar])
                    for c in range(4):
                        S.op("dve", lambda e: e.tensor_tensor(out=q[:, c, :], in0=cv[:, c, :], in1=mu[:], op=ALU.subtract), reads=[Bcv, Bmu], writes=[Bq])
                        S.op("dve", lambda e: e.tensor_tensor(out=q[:, c, :], in0=q[:, c, :], in1=var[:], op=ALU.mult), reads=[Bq, Bvar], writes=[Bq])
                        S.op("act", lambda e: e.activation(out=q[:, c, :], in_=q[:, c, :], func=AF.Silu, scale=cols[:, 12 + c:13 + c], bias=cols[:, 16 + c:17 + c]),
                             reads=[Bq, Bcols], writes=[Bq])
                    S.op("act", lambda e: e.activation(out=q2[:], in_=q[:], func=AF.Square), reads=[Bq], writes=[Bq2])
                    ps, Bps = bsum_bc(q2, Bq2, 4)
                    r, Br = rstd_bc(ps, Bps, 512, 1e-6)
                    for c in range(4):
                        S.op("dve", lambda e: e.scalar_tensor_tensor(out=mg_[:, 4 + c, :], in0=q[:, c, :], scalar=cols[:, 24 + c:25 + c], in1=r[:], op0=ALU.mult, op1=ALU.mult),
                             reads=[Bq, Bcols, Br], writes=[Bmg_])
                    for tl in range(4):
                        xt, Bxt = xr.get()
                        r0 = sti * 512 + tl * 128
                        S.dma("act", xt[:], x_own[r0:r0 + 128, :], writes=[Bxt])
                        x1t, Bx1t = x1r.get()
                        for nh in range(2):
                            ps, Bps = banks.get()
                            for k in range(8):
                                S.op("pe", lambda e, k=k: e.matmul(ps[:], lhsT=mg_[:, k, tl * 128:(tl + 1) * 128], rhs=woutb[:, k, nh * 512:(nh + 1) * 512], start=(k == 0), stop=(k == 7)),
                                     reads=[Bmg_, Bwout], writes=[Bps])
                            S.op("dve", lambda e: e.tensor_tensor(out=x1t[:, nh * 512:(nh + 1) * 512], in0=ps[:], in1=mod[:, 2048 + nh * 512:2048 + (nh + 1) * 512], op=ALU.mult),
                                 reads=[Bps, Bmod], writes=[Bx1t])
                        S.op("dve", lambda e: e.tensor_tensor(out=x1t[:], in0=x1t[:], in1=xt[:], op=ALU.add), reads=[Bx1t, Bxt], writes=[Bx1t])
                        r0 = sti * 512 + tl * 128
                        S.dma("sp", x1s[r0:r0 + 128, :], x1t[:], reads=[Bx1t], writes=[Bx1s[sti]], key=Bx1t)
                        norm_mod_T(x1t, Bx1t, 4096, 3072, h2T, Bh2T, tl, store=sti * 4 + tl)
                    for tl in range(4):
                        ps, Bps = banks.get()
                        for k in range(8):
                            S.op("pe", lambda e, k=k: e.matmul(ps[:, 0:32], lhsT=h2T[:, k, tl * 128:(tl + 1) * 128], rhs=rwb[:, k, :], start=(k == 0), stop=(k == 7)),
                                 reads=[Bh2T, Brw], writes=[Bps])
                        S.op("dve", lambda e: e.tensor_tensor(out=lg[:], in0=ps[:, 0:32], in1=rbb[:], op=ALU.add), reads=[Bps, Brbb], writes=[Blg])
                        S.op("dve", lambda e: e.max(out=mx8[:], in_=lg[:]), reads=[Blg], writes=[Bmx8])
                        S.op("dve", lambda e: e.tensor_scalar(out=msk[:], in0=lg[:], scalar1=mx8[:, 3:4], scalar2=None, op0=ALU.is_ge), reads=[Blg, Bmx8], writes=[Bmsk])
                        tix = sti * 4 + tl
                        S.op("pool", lambda e: e.tensor_copy(out=lgs[:, tix, :], in_=lg[:]), reads=[Blg], writes=[Blgs])
                        S.op("pool", lambda e: e.tensor_copy(out=mx4[:, tix, :], in_=mx8[:, 0:4]), reads=[Bmx8], writes=[Bmx4])
                        S.op("pool", lambda e: e.tensor_copy(out=mskb[:], in_=msk[:]), reads=[Bmsk], writes=[Bmskb])
                        psr, Bpsr = banks.get()
                        S.op("pe", lambda e: e.matmul(psr[:, 0:32], lhsT=ustb[:], rhs=mskb[:], start=True, stop=True), reads=[Bustb, Bmskb], writes=[Bpsr])
                        S.op("pe", lambda e: e.matmul(psr[:, 32:64], lhsT=onesb[:], rhs=mskb[:], start=True, stop=True), reads=[Bones, Bmskb], writes=[Bpsr])
                        S.op("dve", lambda e: e.tensor_tensor(out=rank[:, tix, :], in0=psr[:, 0:32], in1=cum[:, 0, :], op=ALU.add), reads=[Bpsr, Bcum], writes=[Brank])
                        S.op("dve", lambda e: e.tensor_tensor(out=cum[:, 0, :], in0=psr[:, 32:64], in1=cum[:, 0, :], op=ALU.add), reads=[Bpsr, Bcum], writes=[Bcum])
                        S.op("dve", lambda e: e.tensor_scalar(out=sm4[:, 0:1], in0=mx8[:, 0:1], scalar1=-1.0, scalar2=None, op0=ALU.mult), reads=[Bmx8], writes=[Bsm4])
                        S.op("act", lambda e: e.activation(out=lg[:], in_=lg[:], func=AF.Exp, bias=sm4[:, 0:1]), reads=[Blg, Bsm4], writes=[Blg])
                        S.op("dve", lambda e: e.tensor_tensor(out=lg[:], in0=lg[:], in1=msk[:], op=ALU.mult), reads=[Blg, Bmsk], writes=[Blg])
                        S.op("dve", lambda e: e.tensor_reduce(out=sm4[:, 1:2], in_=lg[:], axis=mybir.AxisListType.X, op=ALU.add), reads=[Blg], writes=[Bsm4])
                        S.op("dve", lambda e: e.reciprocal(out=sm4[:, 2:3], in_=sm4[:, 1:2]), reads=[Bsm4], writes=[Bsm4])
                        S.op("dve", lambda e: e.tensor_scalar(out=wts[:, sti * 4 + tl, :], in0=lg[:], scalar1=sm4[:, 2:3], scalar2=None, op0=ALU.mult), reads=[Blg, Bsm4], writes=[Bwts])
                        S.op("act", lambda e: e.activation(out=mx8[:, 4:8], in_=mx8[:, 0:4], func=AF.Exp, bias=sm4[:, 0:1]), reads=[Bmx8, Bsm4], writes=[Bmx8])
                        S.op("dve", lambda e: e.tensor_scalar(out=w4[:, tix, :], in0=mx8[:, 4:8], scalar1=sm4[:, 2:3], scalar2=None, op0=ALU.mult), reads=[Bmx8, Bsm4], writes=[Bw4])

        issue_conv(96)
        S.barrier()
        wg_rows = wgb.rearrange("e (p k) n -> (e p) (k n)", p=128)
        wu_rows = wub.rearrange("e (p k) n -> (e p) (k n)", p=128)
        wd_rows = wdb.rearrange("e (p k) n -> (e p) (k n)", p=128)
        with ExitStack() as me:
            desti, Bdesti = sbt(me, "desti", [128, 4, 32], I32)
            widx, Bwidx = sbt(me, "widx", [128, 2, NSLOT], I32)
            bidx2, Bbidx2 = sbt(me, "bidx2", [128, NSLOT], I32)
            with ExitStack() as dp:
                ci, Bci = sbt(dp, "ci", [128, 32], I32)
                pad, Bpad = sbt(dp, "pad", [128, 32], F32)
                on32, Bon32 = sbt(dp, "on32", [128, 32], F32)
                pend, Bpend = sbt(dp, "pend", [128, 1, 32], F32)
                pst_, Bpst_ = sbt(dp, "pst_", [128, 1, 32], F32)
                Dm, BDm = sbt(dp, "Dm", [128, 32, 32], F32)
                oh, Boh = sbt(dp, "oh", [128, 32, 32], F32)
                dstf, Bdstf = sbt(dp, "dstf", [128, 4, 32], F32)
                jr, Bjr = sbt(dp, "jr", [128, NSLOT, 1], F32)
                pc, Bpc = sbt(dp, "pc", [128, 3], F32)
                cmp_, Bcmp = sbt(dp, "cmp", [128, NSLOT, 32], F32)
                se, Bse = sbt(dp, "se", [128, NSLOT], F32)
                wf, Bwf = sbt(dp, "wf", [128, 2, NSLOT], F32)
                S.dma("sp", jr[:, :, 0], jr_c, writes=[Bjr]); S.dma("sp", pc[:], pcol_c, writes=[Bpc])
                NTH = NTOK // RS + 1
                thr, Bthr = sbt(dp, "thr", [128, 1, NTH], F32)
                cm2, Bcm2 = sbt(dp, "cm2", [128, 32, NTH], F32)
                nbl, Bnbl = sbt(dp, "nbl", [128, 32], F32)
                S.dma("sp", thr[:, 0, :], thr_c, writes=[Bthr])
                S.op("dve", lambda e: e.tensor_tensor(out=cm2[:], in0=thr[:].to_broadcast([128, 32, NTH]), in1=cum[:, 0, :].rearrange("p (e o) -> p e o", o=1).to_broadcast([128, 32, NTH]), op=ALU.is_lt),
                     reads=[Bthr, Bcum], writes=[Bcm2])
                S.op("dve", lambda e: e.tensor_reduce(out=nbl[:], in_=cm2[:], axis=mybir.AxisListType.X, op=ALU.add), reads=[Bcm2], writes=[Bnbl])
                S.op("dve", lambda e: e.tensor_scalar(out=pad[:], in0=nbl[:], scalar1=float(RS), scalar2=None, op0=ALU.mult), reads=[Bnbl], writes=[Bpad])
                S.op("dve", lambda e: e.tensor_copy(out=ci[:], in_=cum[:, 0, :]), reads=[Bcum], writes=[Bci])
                S.op("dve", lambda e: e.tensor_scalar(out=ci[:], in0=ci[:], scalar1=RS - 1, scalar2=None, op0=ALU.add), reads=[Bci], writes=[Bci])
                sh = int(math.log2(RS))
                S.op("dve", lambda e: e.tensor_scalar(out=ci[:], in0=ci[:], scalar1=sh, scalar2=sh, op0=ALU.arith_shift_right, op1=ALU.arith_shift_left), reads=[Bci], writes=[Bci])
                S.op("pool", lambda e: e.memset(on32[:], 1.0), writes=[Bon32])
                S.op("dve", lambda e: e.tensor_tensor_scan(out=pend[:, 0, :], data0=on32[:], data1=pad[:], initial=0.0, op0=ALU.mult, op1=ALU.add), reads=[Bon32, Bpad], writes=[Bpend])
                S.op("dve", lambda e: e.tensor_tensor(out=pst_[:, 0, :], in0=pend[:, 0, :], in1=pad[:], op=ALU.subtract), reads=[Bpend, Bpad], writes=[Bpst_])
                S.op("dve", lambda e: e.tensor_tensor(out=Dm[:], in0=rank[:], in1=pst_[:].to_broadcast([128, 32, 32]), op=ALU.add), reads=[Brank, Bpst_], writes=[BDm])
                for k in range(4):
                    S.op("dve", lambda e: e.tensor_tensor(out=oh[:], in0=lgs[:], in1=mx4[:, :, k:k + 1].to_broadcast([128, 32, 32]), op=ALU.is_equal), reads=[Blgs, Bmx4], writes=[Boh])
                    S.op("dve", lambda e: e.tensor_tensor(out=oh[:], in0=oh[:], in1=Dm[:], op=ALU.mult), reads=[Boh, BDm], writes=[Boh])
                    S.op("dve", lambda e: e.tensor_reduce(out=dstf[:, k, :], in_=oh[:], axis=mybir.AxisListType.X, op=ALU.add), reads=[Boh], writes=[Bdstf])
                S.op("dve", lambda e: e.tensor_copy(out=desti[:], in_=dstf[:]), reads=[Bdstf], writes=[Bdesti])
                S.op("dve", lambda e: e.tensor_tensor(out=cmp_[:], in0=pend[:].to_broadcast([128, NSLOT, 32]), in1=jr[:].to_broadcast([128, NSLOT, 32]), op=ALU.is_le), reads=[Bpend, Bjr], writes=[Bcmp])
                S.op("dve", lambda e: e.tensor_reduce(out=se[:], in_=cmp_[:], axis=mybir.AxisListType.X, op=ALU.add), reads=[Bcmp], writes=[Bse])
                S.op("dve", lambda e: e.tensor_scalar(out=se[:], in0=se[:], scalar1=31.0, scalar2=None, op0=ALU.min), reads=[Bse], writes=[Bse])
                S.op("dve", lambda e: e.tensor_scalar(out=wf[:, 0, :], in0=se[:], scalar1=128.0, scalar2=pc[:, 2:3], op0=ALU.mult, op1=ALU.add), reads=[Bse, Bpc], writes=[Bwf])
                S.op("dve", lambda e: e.tensor_copy(out=bidx2[:], in_=wf[:, 0, :]), reads=[Bwf], writes=[Bbidx2])
                for h in range(2):
                    S.op("dve", lambda e: e.tensor_scalar(out=wf[:, h, :], in0=se[:], scalar1=256.0, scalar2=pc[:, h:h + 1], op0=ALU.mult, op1=ALU.add), reads=[Bse, Bpc], writes=[Bwf])
                S.op("dve", lambda e: e.tensor_copy(out=widx[:], in_=wf[:]), reads=[Bwf], writes=[Bwidx])
                hrr = ring(dp, "hrr", [128, D], BF16, 4)
                scat_bufs = []
                for t in range(nst * 4):
                    hrow, Bhrow = hrr.get()
                    S.dma("sp", hrow[:], h2rows[t * 128:(t + 1) * 128, :], reads=[Bh2r[t]], writes=[Bhrow])
                    for k in range(4):
                        S.idma(Xs, desti[:, k, t:t + 1], hrow[:], None, NSLOT * RS - 1, reads=[Bhrow, Bdesti], key=Bhrow)
                    scat_bufs.append(Bhrow)
                BXs = Buf("Xs")
                S.wait_all("sp", [b for (_, b) in hrr.items])
                S.wait_all("pool", [b for (_, b) in hrr.items])

            S.barrier()
            nslot = NSLOT if nst == NST else max(4, (nst * 512 * 4) // RS + 32)
            bg_rows = b_gate_r.rearrange("e (p c) -> (e p) c", c=8)
            bu_rows = b_up_r.rearrange("e (p c) -> (e p) c", c=8)
            NR = RS // 128
            with ExitStack() as sl:
                wgr = ring(sl, "wg", [128, 8, D], BF16, 2)
                wur = ring(sl, "wu", [128, 8, D], BF16, 2)
                wdr = ring(sl, "wd", [128, 8, D], BF16, 2)
                bcr = ring(sl, "bc", [128, 16], F32, 3)
                xrr = ring(sl, "xrow", [128, D], BF16, 4)
                hTr = ring(sl, "hTm", [128, 8, RS], BF16, 2)
                aTr = ring(sl, "aT", [128, 8, RS], BF16, 2)
                glr = ring(sl, "gl", [128, RS], F32, 3)
                upr = ring(sl, "up", [128, RS], F32, 3)
                sgr = ring(sl, "sgm", [128, RS], F32, 3)
                yr = ring(sl, "yt", [128, D], F32, 3)
                rows_aps = (wg_rows, wu_rows, wd_rows)

                def issue_gathers(j):
                    st_ = {"w": (wgr.get(), wur.get(), wdr.get()), "bc": bcr.get()}
                    bc, Bbc = st_["bc"]
                    S.idma(bc[:, 0:8], None, bg_rows, bidx2[:, j:j + 1], 0, reads=[Bbidx2], writes=[Bbc], key=Bbc)
                    S.idma(bc[:, 8:16], None, bu_rows, bidx2[:, j:j + 1], 0, reads=[Bbidx2], writes=[Bbc], key=Bbc)
                    for mi in range(3):
                        wt_, Bwt_ = st_["w"][mi]
                        S.idma(wt_[:].rearrange("p k n -> p (k n)"), None, rows_aps[mi], bidx2[:, j:j + 1], 0, reads=[Bbidx2] + Bwconv_all, writes=[Bwt_], key=Bwt_)
                    return st_

                def load_rows(j):
                    hT, BhT = hTr.get()
                    for r in range(NR):
                        xrow, Bxrow = xrr.get()
                        r0 = j * RS + r * 128
                        S.dma("sp", xrow[:], Xs[r0:r0 + 128, :], writes=[Bxrow])
                        pst, Bpst = banks.get()
                        pv = pst[:].bitcast(BF16)
                        xv = xrow[:].rearrange("p (c k) -> p k c", k=8)
                        for k in range(8):
                            S.op("pe", lambda e: e.transpose(out=pv[:, k * 128:(k + 1) * 128], in_=xv[:, k, :], identity=idb[:]), reads=[Bxrow, Bidb], writes=[Bpst])
                        S.op("act", lambda e: e.activation(out=hT[:, :, r * 128:(r + 1) * 128], in_=pv[:, 0:1024].rearrange("p (k t) -> p k t", k=8), func=AF.Identity),
                             reads=[Bpst], writes=[BhT])
                    return hT, BhT

                cur = issue_gathers(0)
                cur_h = load_rows(0)
                for j in range(nslot):
                    nxt = issue_gathers(j + 1) if j + 1 < nslot else None
                    (wg, Bwg), (wu, Bwu), (wd, Bwd) = cur["w"]
                    bc, Bbc = cur["bc"]
                    S.op("dve", lambda e: e.tensor_scalar(out=bc[:, 8:16], in0=bc[:, 8:16], scalar1=1.0, scalar2=None, op0=ALU.add), reads=[Bbc], writes=[Bbc])
                    hT, BhT = cur_h
                    aT, BaT = aTr.get()
                    wgv = [wg[:, k, :].rearrange("p (m c) -> p c m", c=8) for k in range(8)]
                    wuv = [wu[:, k, :].rearrange("p (m c) -> p c m", c=8) for k in range(8)]
                    for fc in range(8):
                        psg, Bpsg = banks.get()
                        for k in range(8):
                            S.op("pe", lambda e: e.matmul(psg[:, 0:RS], lhsT=wgv[k][:, fc, :], rhs=hT[:, k, :], start=(k == 0), stop=(k == 7)), reads=[Bwg, BhT], writes=[Bpsg])
                        psu, Bpsu = banks.get()
                        for k in range(8):
                            S.op("pe", lambda e: e.matmul(psu[:, 0:RS], lhsT=wuv[k][:, fc, :], rhs=hT[:, k, :], start=(k == 0), stop=(k == 7)), reads=[Bwu, BhT], writes=[Bpsu])
                        gl, Bgl = glr.get(); up, Bup = upr.get(); sgm, Bsgm = sgr.get()
                        S.op("dve", lambda e: e.tensor_scalar(out=gl[:], in0=psg[:, 0:RS], scalar1=bc[:, fc:fc + 1], scalar2=7.0, op0=ALU.add, op1=ALU.min), reads=[Bpsg, Bbc], writes=[Bgl])
                        S.op("act", lambda e: e.activation(out=sgm[:], in_=gl[:], func=AF.Silu, scale=1.702), reads=[Bgl], writes=[Bsgm])
                        S.op("dve", lambda e: e.tensor_scalar(out=up[:], in0=psu[:, 0:RS], scalar1=bc[:, 8 + fc:9 + fc], scalar2=8.0, op0=ALU.add, op1=ALU.min), reads=[Bpsu, Bbc], writes=[Bup])
                        S.op("dve", lambda e: e.scalar_tensor_tensor(out=aT[:, fc, :], in0=up[:], scalar=-6.0, in1=sgm[:], op0=ALU.max, op1=ALU.mult), reads=[Bup, Bsgm], writes=[BaT])
                    if j + 1 < nslot:
                        cur_h = load_rows(j + 1)
                    for r in range(NR):
                        yt, Byt = yr.get()
                        for nh in range(2):
                            ps, Bps = banks.get()
                            for k in range(8):
                                S.op("pe", lambda e: e.matmul(ps[:], lhsT=aT[:, k, r * 128:(r + 1) * 128], rhs=wd[:, k, nh * 512:(nh + 1) * 512], start=(k == 0), stop=(k == 7)),
                                     reads=[BaT, Bwd], writes=[Bps])
                            S.op("act", lambda e: e.activation(out=yt[:, nh * 512:(nh + 1) * 512], in_=ps[:], func=AF.Copy, scale=1.0 / 1.702), reads=[Bps], writes=[Byt])
                        r0 = j * RS + r * 128
                        S.dma("sp", Ys[r0:r0 + 128, :], yt[:], reads=[Byt], key=Byt)
                    cur = nxt
                S.wait_all("pool", [b for (_, b) in yr.items])

            S.barrier()
            with ExitStack() as cb:
                ykr = ring(cb, "yk", [128, D], F32, 8)
                fgb2, Bfgb2 = sbt(cb, "fgb2", [128, D], F32)
                bdn, Bbdn = sbt(cb, "bdn", [32, D], F32)
                S.dma("sp", fgb2[:], fg_bc, writes=[Bfgb2]); S.dma("sp", bdn[:], b_down, writes=[Bbdn])
                acc, Bacc = sbt(cb, "acc", [128, D], F32)
                wT, BwT = sbt(cb, "wT", [32, 128], F32)
                x1r2 = ring(cb, "x1m", [128, D], F32, 2)
                sq2, Bsq2 = sbt(cb, "sq2", [128, D], F32)
                c4r = ring(cb, "c4", [128, 4], F32, 4)
                for t in range(nst * 4):
                    sti = t // 4
                    pst, Bpst = banks.get()
                    S.op("pe", lambda e: e.transpose(out=pst[0:32, 0:128], in_=wts[:, t, :], identity=idf[:]), reads=[Bwts, Bidf], writes=[Bpst])
                    S.op("act", lambda e: e.activation(out=wT[:], in_=pst[0:32, 0:128], func=AF.Identity), reads=[Bpst], writes=[BwT])
                    for nh in range(2):
                        ps, Bps = banks.get()
                        S.op("pe", lambda e: e.matmul(ps[:], lhsT=wT[:], rhs=bdn[:, nh * 512:(nh + 1) * 512], start=True, stop=True), reads=[BwT, Bbdn], writes=[Bps])
                        S.op("act", lambda e: e.activation(out=acc[:, nh * 512:(nh + 1) * 512], in_=ps[:], func=AF.Identity), reads=[Bps], writes=[Bacc])
                    for k in range(4):
                        yk, Byk = ykr.get()
                        S.idma(yk[:], None, Ys, desti[:, k, t:t + 1], NSLOT * RS - 1, reads=[Bdesti], writes=[Byk], key=Byk)
                        S.op("dve", lambda e: e.scalar_tensor_tensor(out=acc[:], in0=yk[:], scalar=w4[:, t, k:k + 1], in1=acc[:], op0=ALU.mult, op1=ALU.add),
                             reads=[Byk, Bw4, Bacc], writes=[Bacc])
                    x1t, Bx1t = x1r2.get()
                    r0 = t * 128
                    S.dma("sp", x1t[:], x1s[r0:r0 + 128, :], reads=[Bx1s[sti]], writes=[Bx1t])
                    S.op("dve", lambda e: e.tensor_tensor(out=acc[:], in0=acc[:], in1=g2b[:], op=ALU.mult), reads=[Bacc, Bg2b], writes=[Bacc])
                    S.op("dve", lambda e: e.tensor_tensor(out=x1t[:], in0=x1t[:], in1=acc[:], op=ALU.add), reads=[Bx1t, Bacc], writes=[Bx1t])
                    col, Bc = c4r.get()
                    S.op("act", lambda e: e.activation(out=sq2[:], in_=x1t[:], func=AF.Square, accum_out=col[:, 1:2]), reads=[Bx1t], writes=[Bsq2, Bc])
                    S.op("dve", lambda e: e.tensor_scalar(out=col[:, 2:3], in0=col[:, 1:2], scalar1=1.0 / D, scalar2=1e-6, op0=ALU.mult, op1=ALU.add), reads=[Bc], writes=[Bc])
                    S.op("act", lambda e: e.activation(out=col[:, 3:4], in_=col[:, 2:3], func=AF.Sqrt), reads=[Bc], writes=[Bc])
                    S.op("dve", lambda e: e.reciprocal(out=col[:, 0:1], in_=col[:, 3:4]), reads=[Bc], writes=[Bc])
                    S.op("dve", lambda e: e.scalar_tensor_tensor(out=x1t[:], in0=x1t[:], scalar=col[:, 0:1], in1=fgb2[:], op0=ALU.mult, op1=ALU.mult), reads=[Bx1t, Bc, Bfgb2], writes=[Bx1t])
                    S.dma("sp", out[r0:r0 + 128, :], x1t[:], reads=[Bx1t], key=Bx1t)
                S.wait_all("sp", [b for (_, b) in x1r2.items])
    return nc


_CFG = {"nst": NST, "nexp": NEXP, "nprev": NST}


def _host_inputs(inp):
    f = lambda a: np.ascontiguousarray(np.asarray(a, dtype=np.float32))
    x = f(inp["x"]); c = f(inp["c"])
    rep128 = lambda v: f(np.broadcast_to(np.asarray(v, np.float32).reshape(1, -1), (128, np.asarray(v).size)))
    col = lambda v, k: f(np.asarray(v, np.float32).reshape(k, 128).T)
    lam_re = f(inp["lam_re"][0]); lam_im = f(inp["lam_im"][0]); log_dt = f(inp["log_dt"][0])
    b_re = f(inp["b_re"][0]); b_im = f(inp["b_im"][0]); c_re = f(inp["c_re"][0]); c_im = f(inp["c_im"][0])
    def layB_lam(v):
        a = v.reshape(4, 8, 1, 64)
        return f(np.broadcast_to(a, (4, 8, 16, 64)).transpose(1, 2, 0, 3).reshape(128, 4, 64))
    def layB_b(v):
        return f(v.reshape(4, 8, 64, 16).transpose(1, 3, 0, 2).reshape(128, 4, 64))
    dt_full = np.broadcast_to(log_dt.reshape(32, 1), (32, 64))
    def layA_lam(v):
        return f(v.reshape(16, 2, 64).transpose(1, 2, 0).reshape(128, 16))
    def layA_c(v):
        return f(v.reshape(16, 2, 16, 64).transpose(1, 3, 0, 2).reshape(128, 16, 16))
    maskB = np.zeros((128, 8), np.float32)
    for gl in range(8):
        maskB[gl * 16:(gl + 1) * 16, gl] = 1.0
    shared = {
        "ada_w": f(inp["ada_w"][0]), "ada_b_bc": rep128(inp["ada_b"][0]),
        "n1g_bc": rep128(inp["norm1_g"][0]), "n2g_bc": rep128(inp["norm2_g"][0]), "fg_bc": rep128(inp["final_g"]),
        "w_in": f(inp["w_in"][0]), "w_out": f(inp["w_out"][0]), "w_glu": f(inp["w_glu"][0]),
        "router_w": f(inp["router_w"][0]), "router_b_bc": rep128(inp["router_b"][0]),
        "b_glu_col": col(inp["b_glu"][0], 4), "d_col": col(inp["d_skip"][0], 4), "conv_b_col": col(inp["conv_b"][0], 4),
        "ln_g_col": col(inp["ln_g"][0], 4), "ln_b_col": col(inp["ln_b"][0], 4), "og_col": col(inp["out_norm_g"][0], 8),
        "conv_wT": f(np.asarray(inp["conv_w"][0], np.float32).reshape(31, 4, 128).transpose(2, 1, 0)),
        "lamB_re": layB_lam(lam_re), "lamB_im": layB_lam(lam_im), "dtB": layB_lam(dt_full),
        "bB_re": layB_b(b_re), "bB_im": layB_b(b_im), "cA_re": layA_c(c_re), "cA_im": layA_c(c_im),
        "lamA_re": layA_lam(lam_re), "lamA_im": layA_lam(lam_im), "dtA": layA_lam(dt_full), "maskB": maskB,
        "b_gate_r": f(inp["b_gate"][0]), "b_up_r": f(inp["b_up"][0]),
        "ustrict": f(np.triu(np.ones((128, 128), np.float32), 1)),
        "thr_c": f(np.broadcast_to((np.arange(NTOK // RS + 1, dtype=np.float32) * RS).reshape(1, -1), (128, NTOK // RS + 1))),
        "jr_c": f(np.broadcast_to((np.arange(NSLOT, dtype=np.float32) * RS).reshape(1, -1), (128, NSLOT))),
        "pcol_c": f(np.stack([2.0 * np.arange(128), 2.0 * np.arange(128) + 1.0, 1.0 * np.arange(128)], axis=1)),
        "b_down": f(inp["b_down"][0]),
        "w_gate": f(inp["w_gate"][0]), "w_up": f(inp["w_up"][0]), "w_down": f(inp["w_down"][0]),
    }
    maps = []
    for core in range(8):
        b, hf = core // 2, core % 2
        m = dict(shared)
        m["x_own"] = f(x[b, hf * NTOK:(hf + 1) * NTOK])
        m["x_prev"] = f(x[b, 0:NTOK]) if hf == 1 else np.zeros((NTOK, D), np.float32)
        m["flag"] = np.full((128, 1), float(hf), np.float32)
        m["c_col"] = col(c[b], 8)
        maps.append(m)
    return maps


def kernel(**inputs):
    maps = _host_inputs(inputs)
    nc = build_nc(_CFG["nst"], _CFG["nexp"], _CFG["nprev"])
    res = run_bass_kernel_spmd(nc, maps, core_ids=list(range(8)))
    out = np.zeros((4, 2 * NTOK, D), np.float32)
    for core in range(8):
        b, hf = core // 2, core % 2
        out[b, hf * NTOK:(hf + 1) * NTOK] = np.asarray(res.results[core]["out"], np.float32)
    return out
```

```python
import math
import numpy as np
from contextlib import ExitStack
import concourse.bass as bass
import concourse.mybir as mybir
from concourse.bass_utils import run_bass_kernel_spmd

F32 = mybir.dt.float32
BF16 = mybir.dt.bfloat16
I32 = mybir.dt.int32
AF = mybir.ActivationFunctionType
ALU = mybir.AluOpType

D = 1024
NTOK = 4096
NST = 8
NEXP = 32
TS = 128
TWO_PI = 2.0 * math.pi
RS = 384
NSLOT = (NTOK * 4) // RS + 32


class Buf:
    __slots__ = ("name", "w", "r", "sem", "semv")

    def __init__(self, name):
        self.name = name
        self.w = None
        self.r = []
        self.sem = None
        self.semv = 0


class Sched:
    SEM_LIMIT = 30000

    def __init__(self, nc, stack):
        self.nc = nc
        self.stack = stack
        self.engs = {"pe": nc.tensor, "act": nc.scalar, "dve": nc.vector, "pool": nc.gpsimd, "sp": nc.sync}
        self.esem = {}
        self.ecnt = {}
        self.waited = {}
        self.nsem = 0
        self.pesems = set()
        self.dsems = {}
        self.allsems = []
        for e in ("pe", "act", "dve", "pool"):
            self.esem[e] = self.new_sem(e)
            self.ecnt[e] = 0
        self.pesems.add(id(self.esem["pe"]))

    def new_sem(self, name):
        self.nsem += 1
        return self.stack.enter_context(self.nc.semaphore(f"s{self.nsem}_{name}"))

    def barrier(self):
        evs = [(self.esem[e], self.ecnt[e]) for e in ("pe", "act", "dve", "pool") if self.ecnt[e] > 0]
        evs += list(self.dsems.values())
        for e in ("pe", "act", "dve", "pool", "sp"):
            for ev in evs:
                if e != "sp" and ev[0] is self.esem.get(e):
                    continue
                self._wait(e, ev)

    def _wait(self, e, ev):
        if ev is None:
            return
        sem, val = ev
        if e == "pe" and id(sem) in self.pesems:
            return
        key = (e, id(sem))
        if self.waited.get(key, 0) >= val:
            return
        self.waited[key] = val
        self.engs[e].wait_ge(sem, val)

    def _deps(self, e, reads, writes):
        for b in reads:
            self._wait(e, b.w)
        for b in writes:
            self._wait(e, b.w)
            for ev in b.r:
                self._wait(e, ev)

    def _commit(self, ev, reads, writes):
        for b in reads:
            b.r.append(ev)
            if len(b.r) > 64:
                last = {}
                for s, v in b.r:
                    if id(s) not in last or last[id(s)][1] < v:
                        last[id(s)] = (s, v)
                b.r = list(last.values())
        for b in writes:
            b.w = ev
            b.r = []

    def op(self, e, fn, reads=(), writes=()):
        self._deps(e, reads, writes)
        ins = fn(self.engs[e])
        if self.ecnt[e] >= self.SEM_LIMIT:
            self.esem[e] = self.new_sem(e)
            self.ecnt[e] = 0
            if e == "pe":
                self.pesems.add(id(self.esem[e]))
        self.ecnt[e] += 1
        ev = (self.esem[e], self.ecnt[e])
        ins.then_inc(self.esem[e], 1)
        self._commit(ev, reads, writes)
        return ins

    def dma(self, q, out, in_, reads=(), writes=(), key=None):
        self._deps(q, reads, writes)
        kb = key if key is not None else (writes[0] if writes else reads[0])
        if kb.sem is None:
            kb.sem = self.new_sem("d_" + kb.name)
            kb.semv = 0
        ins = self.engs[q].dma_start(out=out, in_=in_)
        kb.semv += 16
        ins.then_inc(kb.sem, 16)
        self.dsems[id(kb.sem)] = (kb.sem, kb.semv)
        self._commit((kb.sem, kb.semv), reads, writes)
        return ins

    def idma(self, out, out_off, in_, in_off, bound, reads=(), writes=(), key=None):
        self._deps("pool", reads, writes)
        kb = key
        if kb.sem is None:
            kb.sem = self.new_sem("d_" + kb.name)
            kb.semv = 0
        oo = bass.IndirectOffsetOnAxis(ap=out_off, axis=0) if out_off is not None else None
        io = bass.IndirectOffsetOnAxis(ap=in_off, axis=0) if in_off is not None else None
        try:
            ins = self.nc.gpsimd.indirect_dma_start(out=out, out_offset=oo, in_=in_, in_offset=io)
        except Exception:
            print("IDMA FAIL", out, in_, out_off, in_off, bound)
            raise
        kb.semv += 16
        ins.then_inc(kb.sem, 16)
        self.dsems[id(kb.sem)] = (kb.sem, kb.semv)
        self._commit((kb.sem, kb.semv), reads, writes)
        return ins

    def wait_all(self, e, bufs):
        for b in bufs:
            self._wait(e, b.w)
            for ev in b.r:
                self._wait(e, ev)


class Ring:
    def __init__(self, items):
        self.items = items
        self.i = 0

    def get(self):
        it = self.items[self.i % len(self.items)]
        self.i += 1
        return it


def build_nc(nst=NST, nexp=NEXP, nprev=NST):
    nc = bass.Bass("TRN2", target_bir_lowering=False)
    dt_in = lambda n, s: nc.dram_tensor(n, s, F32, kind="ExternalInput").ap()
    x_own = dt_in("x_own", [NTOK, D]); x_prev = dt_in("x_prev", [NTOK, D]); flag = dt_in("flag", [128, 1])
    c_col = dt_in("c_col", [128, 8]); ada_w = dt_in("ada_w", [D, 6 * D]); ada_b_bc = dt_in("ada_b_bc", [128, 6 * D])
    n1g_bc = dt_in("n1g_bc", [128, D]); n2g_bc = dt_in("n2g_bc", [128, D]); fg_bc = dt_in("fg_bc", [128, D])
    w_in = dt_in("w_in", [D, 1536]); w_out = dt_in("w_out", [D, D]); w_glu = dt_in("w_glu", [512, 512])
    router_w = dt_in("router_w", [D, 32]); router_b_bc = dt_in("router_b_bc", [128, 32])
    b_glu_col = dt_in("b_glu_col", [128, 4]); d_col = dt_in("d_col", [128, 4]); conv_b_col = dt_in("conv_b_col", [128, 4])
    ln_g_col = dt_in("ln_g_col", [128, 4]); ln_b_col = dt_in("ln_b_col", [128, 4]); og_col = dt_in("og_col", [128, 8])
    conv_wT = dt_in("conv_wT", [128, 4, 31])
    lamB_re = dt_in("lamB_re", [128, 4, 64]); lamB_im = dt_in("lamB_im", [128, 4, 64]); dtB = dt_in("dtB", [128, 4, 64])
    bB_re = dt_in("bB_re", [128, 4, 64]); bB_im = dt_in("bB_im", [128, 4, 64])
    cA_re = dt_in("cA_re", [128, 16, 16]); cA_im = dt_in("cA_im", [128, 16, 16])
    lamA_re = dt_in("lamA_re", [128, 16]); lamA_im = dt_in("lamA_im", [128, 16]); dtA = dt_in("dtA", [128, 16])
    maskB = dt_in("maskB", [128, 8])
    b_gate_r = dt_in("b_gate_r", [32, D]); b_up_r = dt_in("b_up_r", [32, D]); b_down = dt_in("b_down", [32, D])
    w_gate = dt_in("w_gate", [32, D, D]); w_up = dt_in("w_up", [32, D, D]); w_down = dt_in("w_down", [32, D, D])
    out = nc.dram_tensor("out", [NTOK, D], F32, kind="ExternalOutput").ap()
    thr_c = dt_in("thr_c", [128, NTOK // RS + 1]); ustrict = dt_in("ustrict", [128, 128]); jr_c = dt_in("jr_c", [128, NSLOT]); pcol_c = dt_in("pcol_c", [128, 3])
    x1s = nc.dram_tensor("x1s", [NTOK, D], F32, kind="Internal").ap()
    h2rows = nc.dram_tensor("h2rows", [NTOK, D], BF16, kind="Internal").ap()
    Xs = nc.dram_tensor("Xs", [NSLOT * RS, D], BF16, kind="Internal").ap()
    Ys = nc.dram_tensor("Ys", [NSLOT * RS, D], F32, kind="Internal").ap()
    wgb = nc.dram_tensor("wgb", [32, D, D], BF16, kind="Internal").ap()
    wub = nc.dram_tensor("wub", [32, D, D], BF16, kind="Internal").ap()
    wdb = nc.dram_tensor("wdb", [32, D, D], BF16, kind="Internal").ap()
    Bwconv = [[Buf(f"wcv{m}_{q}") for q in range(32)] for m in range(3)]
    Bcvkey = Buf("cvkey")
    Bwconv_all = Bwconv[0] + Bwconv[1] + Bwconv[2]
    Bx1s = [Buf(f"x1s{i}") for i in range(NST)]
    Bh2r = [Buf(f"h2r{i}") for i in range(NST * 4)]

    with ExitStack() as top:
        S = Sched(nc, top)

        def sbt(st, name, shape, dt):
            return st.enter_context(nc.sbuf_tensor(name, shape, dt)), Buf(name)

        def ring(st, name, shape, dt, n):
            return Ring([sbt(st, f"{name}{i}", shape, dt) for i in range(n)])

        banks = Ring([(top.enter_context(nc.psum_tensor(f"bank{i}", [128, 512], F32)), Buf(f"bank{i}")) for i in range(5)])
        cbank = (top.enter_context(nc.psum_tensor("cbank", [128, 512], F32)), Buf("cbank"))
        ybanks = Ring([(top.enter_context(nc.psum_tensor(f"ybank{i}", [128, 512], F32)), Buf(f"ybank{i}")) for i in range(2)])

        idf, Bidf = sbt(top, "idf", [128, 128], F32)
        idb, Bidb = sbt(top, "idb", [128, 128], BF16)
        onesb, Bones = sbt(top, "onesb", [128, 128], BF16)
        wts, Bwts = sbt(top, "wts", [128, 32, 32], F32)
        flg, Bflg = sbt(top, "flg", [128, 1], F32)
        g2b, Bg2b = sbt(top, "g2b", [128, D], BF16)
        lgs, Blgs = sbt(top, "lgs", [128, 32, 32], F32)
        mx4, Bmx4 = sbt(top, "mx4", [128, 32, 4], F32)
        w4, Bw4 = sbt(top, "w4", [128, 32, 4], F32)
        rank, Brank = sbt(top, "rank", [128, 32, 32], F32)
        cum, Bcum = sbt(top, "cum", [128, 1, 32], F32)
        ustb, Bustb = sbt(top, "ustb", [128, 128], BF16)
        S.dma("pool", ustb[:], ustrict, writes=[Bustb])
        S.op("pool", lambda e: e.memset(cum[:], 0.0), writes=[Bcum])
        S.op("pool", lambda e: e.memset(idf[:], 0.0), writes=[Bidf])
        S.op("pool", lambda e: e.affine_select(out=idf[:], in_=idf[:], pattern=[[-1, 128]], compare_op=ALU.not_equal,
                                               fill=1.0, base=0, channel_multiplier=1), reads=[Bidf], writes=[Bidf])
        S.op("dve", lambda e: e.tensor_copy(out=idb[:], in_=idf[:]), reads=[Bidf], writes=[Bidb])
        S.op("pool", lambda e: e.memset(onesb[:], 1.0), writes=[Bones])
        S.dma("sp", flg[:], flag, writes=[Bflg])
        conv_jobs = [(src_, dst_, Bwconv[m], ex) for ex in range(32) for m, (src_, dst_) in enumerate(((w_gate, wgb), (w_up, wub), (w_down, wdb)))]

        def issue_conv(n, pace=()):
            S._deps("pool", list(pace), [])
            for _ in range(n):
                if not conv_jobs:
                    return
                src_, dst_, Bd, ex = conv_jobs.pop(0)
                S.dma("pool", dst_[ex].rearrange("(a b) n -> a (b n)", a=16), src_[ex].rearrange("(a b) n -> a (b n)", a=16), writes=[Bd[ex]], key=Bcvkey)

        def transpose_to(dst_ap, Bdst, src_ap, Bsrc, ident, Bident, evac="act", nblk=1, dt=BF16):
            pst, Bpst = banks.get()
            pv = pst[:].bitcast(dt) if dt != F32 else pst[:]
            for j in range(nblk):
                S.op("pe", lambda e, j=j: e.transpose(out=pv[:, j * 128:(j + 1) * 128], in_=src_ap(j), identity=ident[:]),
                     reads=[Bsrc, Bident], writes=[Bpst])
            if evac == "act":
                S.op("act", lambda e: e.activation(out=dst_ap, in_=pv[:, 0:nblk * 128], func=AF.Identity), reads=[Bpst], writes=[Bdst])
            else:
                S.op(evac, lambda e: e.tensor_copy(out=dst_ap, in_=pv[:, 0:nblk * 128]), reads=[Bpst], writes=[Bdst])

        with ExitStack() as mx:
            mod, Bmod = sbt(mx, "mod", [128, 5 * D], BF16)
            winb, Bwin = sbt(mx, "winb", [128, 8, 1536], BF16)
            woutb, Bwout = sbt(mx, "woutb", [128, 8, D], BF16)
            wglub, Bwglu = sbt(mx, "wglub", [128, 4, 512], BF16)
            rwb, Brw = sbt(mx, "rwb", [128, 8, 32], BF16)
            rbb, Brbb = sbt(mx, "rbb", [128, 32], F32)
            cols, Bcols = sbt(mx, "cols", [128, 32], F32)
            cwt, Bcwt = sbt(mx, "cwt", [128, 4, 31], F32)
            BL, BBL = sbt(mx, "BL", [128, 16, 2, 128], BF16)
            CL, BCL = sbt(mx, "CL", [128, 16, 2, 128], BF16)
            Ec, BEc = sbt(mx, "Ec", [128, 16, TS], F32)
            Es, BEs = sbt(mx, "Es", [128, 16, TS], F32)
            rho, Brho = sbt(mx, "rho", [128, 16], F32)
            rho0, Brho0 = sbt(mx, "rho0", [128, 16, TS], F32)
            ddg, Bddg = sbt(mx, "ddg", [128, 4, 128], BF16)
            sre, Bsre = sbt(mx, "sre", [128, 16], F32)
            sim, Bsim = sbt(mx, "sim", [128, 16], F32)
            zbuf, Bzbuf = sbt(mx, "zbuf", [128, 4, 30 + 512], BF16)

            S.dma("sp", cols[:, 0:4], b_glu_col, writes=[Bcols]); S.dma("sp", cols[:, 4:8], d_col, writes=[Bcols])
            S.dma("sp", cols[:, 8:12], conv_b_col, writes=[Bcols]); S.dma("sp", cols[:, 12:16], ln_g_col, writes=[Bcols])
            S.dma("sp", cols[:, 16:20], ln_b_col, writes=[Bcols]); S.dma("sp", cols[:, 20:28], og_col, writes=[Bcols])
            S.dma("sp", rbb[:], router_b_bc, writes=[Brbb])
            S.dma("pool", winb[:], w_in.rearrange("(k p) n -> p k n", p=128), writes=[Bwin])
            S.dma("pool", woutb[:], w_out.rearrange("(k p) n -> p k n", p=128), writes=[Bwout])
            S.dma("pool", wglub[:], w_glu.rearrange("(k p) n -> p k n", p=128), writes=[Bwglu])
            S.dma("pool", rwb[:], router_w.rearrange("(k p) n -> p k n", p=128), writes=[Brw])
            Bsre_t = [Buf(f"sre_{t}") for t in range(4)]
            Bsim_t = [Buf(f"sim_{t}") for t in range(4)]
            S.op("pool", lambda e: e.memset(sre[:], 0.0), writes=[Bsre] + Bsre_t)
            S.op("pool", lambda e: e.memset(sim[:], 0.0), writes=[Bsim] + Bsim_t)
            S.op("pool", lambda e: e.memset(zbuf[:], 0.0), writes=[Bzbuf])

            with ExitStack() as su:
                ccol, Bccol = sbt(su, "ccol", [128, 8], F32)
                crep, Bcrep = sbt(su, "crep", [128, 8, 128], F32)
                awr = ring(su, "aw", [128, 8, 512], F32, 2)
                abr = ring(su, "ab", [128, 512], F32, 2)
                S.dma("sp", ccol[:], c_col, writes=[Bccol])
                S.op("act", lambda e: e.activation(out=ccol[:], in_=ccol[:], func=AF.Silu), reads=[Bccol], writes=[Bccol])
                for k in range(8):
                    S.op("dve", lambda e, k=k: e.tensor_copy(out=crep[:, k, :], in_=ccol[:, k:k + 1].to_broadcast([128, 128])),
                         reads=[Bccol], writes=[Bcrep])
                awv = ada_w.rearrange("(k p) n -> p k n", p=128)
                for nt in range(12):
                    aw, Baw = awr.get(); ab, Bab = abr.get()
                    S.dma("sp", aw[:], awv[:, :, nt * 512:(nt + 1) * 512], writes=[Baw])
                    S.dma("act", ab[:], ada_b_bc[:, nt * 512:(nt + 1) * 512], writes=[Bab])
                    ps, Bps = banks.get()
                    for k in range(8):
                        S.op("pe", lambda e, k=k: e.matmul(ps[:], lhsT=crep[:, k, :], rhs=aw[:, k, :], start=(k == 0), stop=(k == 7)),
                             reads=[Bcrep, Baw], writes=[Bps])
                    if nt < 10:
                        S.op("dve", lambda e: e.tensor_tensor(out=mod[:, nt * 512:(nt + 1) * 512], in0=ps[:], in1=ab[:], op=ALU.add),
                             reads=[Bps, Bab], writes=[Bmod])
                    else:
                        S.op("dve", lambda e: e.tensor_tensor(out=g2b[:, (nt - 10) * 512:(nt - 9) * 512], in0=ps[:], in1=ab[:], op=ALU.add),
                             reads=[Bps, Bab], writes=[Bg2b])
                gt_, Bgt = sbt(su, "gtmp", [128, D], F32)
                for (gsrc, lo) in ((n1g_bc, 1024), (n2g_bc, 4096)):
                    S.dma("sp", gt_[:], gsrc, writes=[Bgt])
                    S.op("dve", lambda e, lo=lo: e.scalar_tensor_tensor(out=mod[:, lo:lo + D], in0=mod[:, lo:lo + D], scalar=1.0,
                                                                      in1=gt_[:], op0=ALU.add, op1=ALU.mult),
                         reads=[Bmod, Bgt], writes=[Bmod])
                S.dma("sp", cwt[:], conv_wT, writes=[Bcwt])

                def cossin(tag, th, Bth, shape, n):
                    y, By = sbt(su, tag + "y", shape, F32); ki, Bki = sbt(su, tag + "ki", shape, I32)
                    f, Bf = sbt(su, tag + "f", shape, F32); m, Bm = sbt(su, tag + "m", shape, F32)
                    g, Bg = sbt(su, tag + "g", shape, F32)
                    co, Bco = sbt(su, tag + "co", shape, F32); si, Bsi = sbt(su, tag + "si", shape, F32)
                    S.op("dve", lambda e: e.tensor_scalar(out=y[:], in0=th[:], scalar1=1.0 / TWO_PI, scalar2=None, op0=ALU.mult), reads=[Bth], writes=[By])
                    S.op("dve", lambda e: e.tensor_copy(out=ki[:], in_=y[:]), reads=[By], writes=[Bki])
                    S.op("dve", lambda e: e.tensor_copy(out=f[:], in_=ki[:]), reads=[Bki], writes=[Bf])
                    S.op("dve", lambda e: e.tensor_tensor(out=f[:], in0=y[:], in1=f[:], op=ALU.subtract), reads=[By, Bf], writes=[Bf])

                    def wrap(t, Bt):
                        S.op("dve", lambda e: e.tensor_scalar(out=m[:], in0=t[:], scalar1=0.5, scalar2=None, op0=ALU.is_gt), reads=[Bt], writes=[Bm])
                        S.op("dve", lambda e: e.tensor_tensor(out=t[:], in0=t[:], in1=m[:], op=ALU.subtract), reads=[Bt, Bm], writes=[Bt])
                        S.op("dve", lambda e: e.tensor_scalar(out=m[:], in0=t[:], scalar1=-0.5, scalar2=None, op0=ALU.is_lt), reads=[Bt], writes=[Bm])
                        S.op("dve", lambda e: e.tensor_tensor(out=t[:], in0=t[:], in1=m[:], op=ALU.add), reads=[Bt, Bm], writes=[Bt])
                    wrap(f, Bf)
                    S.op("dve", lambda e: e.tensor_scalar(out=g[:], in0=f[:], scalar1=0.25, scalar2=None, op0=ALU.add), reads=[Bf], writes=[Bg])
                    wrap(g, Bg)
                    S.op("act", lambda e: e.activation(out=si[:], in_=f[:], func=AF.Sin, scale=TWO_PI), reads=[Bf], writes=[Bsi])
                    S.op("act", lambda e: e.activation(out=co[:], in_=g[:], func=AF.Sin, scale=TWO_PI), reads=[Bg], writes=[Bco])
                    return (co, Bco), (si, Bsi)

                def abar(tag, lre_d, lim_d, dt_d, shape):
                    lr, Blr = sbt(su, tag + "lr", shape, F32); li, Bli = sbt(su, tag + "li", shape, F32)
                    dv, Bdv = sbt(su, tag + "dv", shape, F32); mg, Bmg = sbt(su, tag + "mg", shape, F32)
                    th, Bth = sbt(su, tag + "th", shape, F32)
                    ar, Bar = sbt(su, tag + "ar", shape, F32); ai, Bai = sbt(su, tag + "ai", shape, F32)
                    S.dma("sp", lr[:], lre_d, writes=[Blr]); S.dma("sp", li[:], lim_d, writes=[Bli]); S.dma("sp", dv[:], dt_d, writes=[Bdv])
                    S.op("act", lambda e: e.activation(out=dv[:], in_=dv[:], func=AF.Exp), reads=[Bdv], writes=[Bdv])
                    S.op("dve", lambda e: e.tensor_tensor(out=mg[:], in0=lr[:], in1=dv[:], op=ALU.mult), reads=[Blr, Bdv], writes=[Bmg])
                    S.op("act", lambda e: e.activation(out=mg[:], in_=mg[:], func=AF.Exp), reads=[Bmg], writes=[Bmg])
                    S.op("dve", lambda e: e.tensor_tensor(out=th[:], in0=li[:], in1=dv[:], op=ALU.mult), reads=[Bli, Bdv], writes=[Bth])
                    (co, Bco), (si, Bsi) = cossin(tag, th, Bth, shape, 0)
                    S.op("dve", lambda e: e.tensor_tensor(out=ar[:], in0=mg[:], in1=co[:], op=ALU.mult), reads=[Bmg, Bco], writes=[Bar])
                    S.op("dve", lambda e: e.tensor_tensor(out=ai[:], in0=mg[:], in1=si[:], op=ALU.mult), reads=[Bmg, Bsi], writes=[Bai])
                    return (ar, Bar), (ai, Bai), (lr, Blr), (li, Bli), (mg, Bmg), (co, Bco), (si, Bsi)

                shB = [128, 4, 64]
                (ar, Bar), (ai, Bai), (lr, Blr), (li, Bli), _, _, _ = abar("B", lamB_re, lamB_im, dtB, shB)
                den, Bden = sbt(su, "den", shB, F32); t1, Bt1 = sbt(su, "t1", shB, F32); t2, Bt2 = sbt(su, "t2", shB, F32)
                qr, Bqr = sbt(su, "qr", shB, F32); qi, Bqi = sbt(su, "qi", shB, F32)
                br_, Bbr = sbt(su, "br_", shB, F32); bi_, Bbi = sbt(su, "bi_", shB, F32)
                bbr, Bbbr = sbt(su, "bbr", shB, F32); bbi, Bbbi = sbt(su, "bbi", shB, F32)
                tt = lambda o, Bo, a, Ba, b, Bb, op: S.op("dve", lambda e: e.tensor_tensor(out=o[:], in0=a[:], in1=b[:], op=op), reads=[Ba, Bb], writes=[Bo])
                tt(den, Bden, lr, Blr, lr, Blr, ALU.mult); tt(t1, Bt1, li, Bli, li, Bli, ALU.mult); tt(den, Bden, den, Bden, t1, Bt1, ALU.add)
                S.op("dve", lambda e: e.reciprocal(out=den[:], in_=den[:]), reads=[Bden], writes=[Bden])
                S.op("dve", lambda e: e.tensor_scalar(out=ar[:], in0=ar[:], scalar1=-1.0, scalar2=None, op0=ALU.add), reads=[Bar], writes=[Bar])
                tt(t1, Bt1, ar, Bar, lr, Blr, ALU.mult); tt(t2, Bt2, ai, Bai, li, Bli, ALU.mult); tt(qr, Bqr, t1, Bt1, t2, Bt2, ALU.add)
                tt(qr, Bqr, qr, Bqr, den, Bden, ALU.mult)
                tt(t1, Bt1, ai, Bai, lr, Blr, ALU.mult); tt(t2, Bt2, ar, Bar, li, Bli, ALU.mult); tt(qi, Bqi, t1, Bt1, t2, Bt2, ALU.subtract)
                tt(qi, Bqi, qi, Bqi, den, Bden, ALU.mult)
                S.dma("sp", br_[:], bB_re, writes=[Bbr]); S.dma("sp", bi_[:], bB_im, writes=[Bbi])
                tt(t1, Bt1, qr, Bqr, br_, Bbr, ALU.mult); tt(t2, Bt2, qi, Bqi, bi_, Bbi, ALU.mult); tt(bbr, Bbbr, t1, Bt1, t2, Bt2, ALU.subtract)
                tt(t1, Bt1, qr, Bqr, bi_, Bbi, ALU.mult); tt(t2, Bt2, qi, Bqi, br_, Bbr, ALU.mult); tt(bbi, Bbbi, t1, Bt1, t2, Bt2, ALU.add)
                mkb, Bmkb = sbt(su, "mkb", [128, 8], F32)
                S.dma("sp", mkb[:], maskB, writes=[Bmkb])
                for pair in range(16):
                    t, pl = pair // 4, pair % 4
                    for ri, (src, Bsrc) in enumerate(((bbr, Bbbr), (bbi, Bbbi))):
                        for half in range(2):
                            S.op("dve", lambda e, pair=pair, ri=ri, half=half, src=src, t=t, pl=pl: e.tensor_scalar(
                                out=BL[:, pair, ri, half * 64:(half + 1) * 64], in0=src[:, t, :],
                                scalar1=mkb[:, 2 * pl + half:2 * pl + half + 1], scalar2=None, op0=ALU.mult),
                                reads=[Bsrc, Bmkb], writes=[BBL])
                car, Bcar = sbt(su, "car", [128, 16, 16], F32); cai, Bcai = sbt(su, "cai", [128, 16, 16], F32)
                S.dma("sp", car[:], cA_re, writes=[Bcar]); S.dma("sp", cai[:], cA_im, writes=[Bcai])
                S.op("pool", lambda e: e.memset(CL[:], 0.0), writes=[BCL])
                for pair in range(16):
                    pl = pair % 4
                    for half in range(2):
                        c0 = 32 * pl + 16 * half
                        S.op("dve", lambda e, pair=pair, half=half, c0=c0: e.tensor_copy(
                            out=CL[half * 64:(half + 1) * 64, pair, 0, c0:c0 + 16], in_=car[half * 64:(half + 1) * 64, pair, :]),
                            reads=[Bcar], writes=[BCL])
                        S.op("dve", lambda e, pair=pair, half=half, c0=c0: e.tensor_scalar(
                            out=CL[half * 64:(half + 1) * 64, pair, 1, c0:c0 + 16], in0=cai[half * 64:(half + 1) * 64, pair, :],
                            scalar1=-1.0, scalar2=None, op0=ALU.mult), reads=[Bcai], writes=[BCL])
                shA = [128, 16]
                _, _, _, _, (mgA, BmgA), (coA, BcoA), (siA, BsiA) = abar("A", lamA_re, lamA_im, dtA, shA)
                S.op("dve", lambda e: e.tensor_copy(out=rho[:], in_=mgA[:]), reads=[BmgA], writes=[Brho])
                S.op("dve", lambda e: e.tensor_copy(out=rho0[:], in_=mgA[:].rearrange("p (a o) -> p a o", o=1).to_broadcast([128, 16, TS])), reads=[BmgA], writes=[Brho0])
                S.op("dve", lambda e: e.memset(rho0[:, :, 0:1], 0.0), reads=[], writes=[Brho0])
                for t in range(4):
                    S.op("dve", lambda e: e.tensor_scalar(out=ddg[:, t, :], in0=idf[:], scalar1=cols[:, 4 + t:5 + t], scalar2=None, op0=ALU.mult), reads=[Bidf, Bcols], writes=[Bddg])
                S.op("dve", lambda e: e.tensor_copy(out=Ec[:, :, 0], in_=coA[:]), reads=[BcoA], writes=[BEc])
                S.op("dve", lambda e: e.tensor_copy(out=Es[:, :, 0], in_=siA[:]), reads=[BsiA], writes=[BEs])
                ta, Bta = sbt(su, "ta", [128, 16, TS // 2], F32); tb, Btb = sbt(su, "tb", [128, 16, TS // 2], F32)
                m = 1
                while m < TS:
                    cm = Ec[:, :, m - 1:m].to_broadcast([128, 16, m]); sm = Es[:, :, m - 1:m].to_broadcast([128, 16, m])
                    S.op("dve", lambda e, m=m, cm=cm: e.tensor_tensor(out=ta[:, :, 0:m], in0=Ec[:, :, 0:m], in1=cm, op=ALU.mult), reads=[BEc], writes=[Bta])
                    S.op("dve", lambda e, m=m, sm=sm: e.tensor_tensor(out=tb[:, :, 0:m], in0=Es[:, :, 0:m], in1=sm, op=ALU.mult), reads=[BEs], writes=[Btb])
                    S.op("dve", lambda e, m=m: e.tensor_tensor(out=ta[:, :, 0:m], in0=ta[:, :, 0:m], in1=tb[:, :, 0:m], op=ALU.subtract), reads=[Bta, Btb], writes=[Bta])
                    S.op("dve", lambda e, m=m, sm=sm: e.tensor_tensor(out=tb[:, :, 0:m], in0=Ec[:, :, 0:m], in1=sm, op=ALU.mult), reads=[BEc, BEs], writes=[Btb])
                    S.op("dve", lambda e, m=m: e.tensor_copy(out=Ec[:, :, m:2 * m], in_=ta[:, :, 0:m]), reads=[Bta], writes=[BEc])
                    S.op("dve", lambda e, m=m, cm=cm: e.tensor_tensor(out=ta[:, :, 0:m], in0=Es[:, :, 0:m], in1=cm, op=ALU.mult), reads=[BEs, BEc], writes=[Bta])
                    S.op("dve", lambda e, m=m: e.tensor_tensor(out=Es[:, :, m:2 * m], in0=ta[:, :, 0:m], in1=tb[:, :, 0:m], op=ALU.add), reads=[Bta, Btb], writes=[BEs])
                    m *= 2

            S.barrier()
            with ExitStack() as ml:
                xr = ring(ml, "xt", [128, D], F32, 2)
                hr = ring(ml, "ht", [128, D], BF16, 1)
                st4 = ring(ml, "st4", [128, 8], F32, 4)
                hT, BhT = sbt(ml, "hT", [128, 8, 512], BF16)
                uTb, BuTb = sbt(ml, "uTb", [128, 4, 512], BF16)
                sg, Bsg = sbt(ml, "sg", [128, 512], BF16)
                Abig = [sbt(ml, f"Abig{i}", [128, 2048], F32) for i in range(4)]
                xbr = ring(ml, "xb", [128, 512], BF16, 2)
                st8, Bst8 = sbt(ml, "st8", [128, 64], F32)
                dgr = ring(ml, "dg", [128, 128], BF16, 4)
                cv, Bcv = sbt(ml, "cv", [128, 4, 512], BF16)
                conv_state = {"todo": []}

                def conv_fill(n):
                    cps, Bcps = cbank
                    for _ in range(n):
                        if not conv_state["todo"]:
                            return
                        c, j = conv_state["todo"].pop(0)
                        dgt, Bdgt = dgr.get()
                        S.op("dve", lambda e: e.tensor_scalar(out=dgt[:], in0=idf[:], scalar1=cwt[:, c, j:j + 1], scalar2=None, op0=ALU.mult), reads=[Bidf, Bcwt], writes=[Bdgt])
                        S.op("pe", lambda e: e.matmul(cps[:], lhsT=dgt[:], rhs=zbuf[:, c, j:j + 512], start=(j == 0), stop=(j == 30)), reads=[Bdgt, Bzbuf], writes=[Bcps])
                        if j == 30:
                            S.op("act", lambda e: e.activation(out=cv[:, c, :], in_=cps[:], func=AF.Identity, bias=cols[:, 8 + c:9 + c]), reads=[Bcps, Bcols], writes=[Bcv])
                Bst8_t = [Buf(f"st8_{t}") for t in range(4)]
                mg_, Bmg_ = sbt(ml, "mg", [128, 8, 512], BF16)
                q, Bq = sbt(ml, "q", [128, 4, 512], F32)
                ys, Bys = q, Bq
                sq, Bsq = q[:, 0:2, :].rearrange("p a b -> p (a b)"), Bq
                Bqc = [Buf(f"qc{c}") for c in range(4)]
                qb, Bqb = sbt(ml, "qb", [128, 4, 512], BF16)
                q2, Bq2 = qb, Bqb
                yg, Byg = qb, Bqb
                nr_ = ring(ml, "nr", [128, 512], F32, 2)
                x1r = ring(ml, "x1t", [128, D], F32, 1)
                h2T, Bh2T = hT, BhT
                lg, Blg = sbt(ml, "lg", [128, 32], F32)
                mx8, Bmx8 = sbt(ml, "mx8", [128, 8], F32)
                msk, Bmsk = sbt(ml, "msk", [128, 32], F32)
                mskb, Bmskb = sbt(ml, "mskb", [128, 32], BF16)
                sm4, Bsm4 = sbt(ml, "sm4", [128, 4], F32)

                def rstd_of(xt, Bxt, col, Bcol, n=D):
                    S.op("act", lambda e: e.activation(out=sq[:, 0:n], in_=xt, func=AF.Square, accum_out=col[:, 1:2]), reads=[Bxt], writes=[Bsq, Bcol])
                    S.op("dve", lambda e: e.tensor_scalar(out=col[:, 2:3], in0=col[:, 1:2], scalar1=1.0 / n, scalar2=1e-6, op0=ALU.mult, op1=ALU.add), reads=[Bcol], writes=[Bcol])
                    S.op("act", lambda e: e.activation(out=col[:, 3:4], in_=col[:, 2:3], func=AF.Sqrt), reads=[Bcol], writes=[Bcol])
                    S.op("dve", lambda e: e.reciprocal(out=col[:, 0:1], in_=col[:, 3:4]), reads=[Bcol], writes=[Bcol])

                def norm_mod_T(xt, Bxt, a_lo, sh_lo, dstT, BdstT, tcol, store=None):
                    col, Bc = st4.get()
                    rstd_of(xt[:], Bxt, col, Bc)
                    h, Bh = hr.get()
                    S.op("dve", lambda e: e.scalar_tensor_tensor(out=sq[:], in0=xt[:], scalar=col[:, 0:1], in1=mod[:, a_lo:a_lo + D], op0=ALU.mult, op1=ALU.mult),
                         reads=[Bxt, Bc, Bmod], writes=[Bsq])
                    S.op("dve", lambda e: e.tensor_tensor(out=h[:], in0=sq[:], in1=mod[:, sh_lo:sh_lo + D], op=ALU.add), reads=[Bsq, Bmod], writes=[Bh])
                    if store is not None:
                        S.dma("sp", h2rows[store * 128:(store + 1) * 128, :], h[:], reads=[Bh], writes=[Bh2r[store]], key=Bh)
                    pst, Bpst = banks.get()
                    pv = pst[:].bitcast(BF16)
                    for k in range(8):
                        S.op("pe", lambda e, k=k: e.transpose(out=pv[:, k * 128:(k + 1) * 128], in_=h[:, k * 128:(k + 1) * 128], identity=idb[:]),
                             reads=[Bh, Bidb], writes=[Bpst])
                    S.op("act", lambda e: e.activation(out=dstT[:, :, tcol * 128:(tcol + 1) * 128], in_=pv[:, 0:1024].rearrange("p (k t) -> p k t", k=8), func=AF.Identity),
                         reads=[Bpst], writes=[BdstT])

                def bsum_bc(src, Bsrc, nch, c0=0):
                    ps, Bps = banks.get()
                    for k in range(nch):
                        S.op("pe", lambda e, k=k: e.matmul(ps[:], lhsT=onesb[:], rhs=src[:, c0 + k, :], start=(k == 0), stop=(k == nch - 1)),
                             reads=[Bones, Bsrc], writes=[Bps])
                    return ps, Bps

                def rstd_bc(ps, Bps, n, eps):
                    r, Br = nr_.get()
                    S.op("dve", lambda e: e.tensor_scalar(out=r[:], in0=ps[:], scalar1=1.0 / n, scalar2=eps, op0=ALU.mult, op1=ALU.add), reads=[Bps], writes=[Br])
                    S.op("act", lambda e: e.activation(out=r[:], in_=r[:], func=AF.Sqrt), reads=[Br], writes=[Br])
                    S.op("dve", lambda e: e.reciprocal(out=r[:], in_=r[:]), reads=[Br], writes=[Br])
                    return r, Br

                def ssm(own):
                    NSC = 512 // TS
                    EcA = Ec[:].rearrange("p a b -> p (a b)"); EsA = Es[:].rearrange("p a b -> p (a b)"); R0A = rho0[:].rearrange("p a b -> p (a b)")
                    (A1, BA1), (A2, BA2), (A3, BA3), (A4, BA4) = Abig
                    A1v = A1[:].rearrange("p (a b) -> p a b", a=16); A3v = A3[:].rearrange("p (a b) -> p a b", a=16)
                    A2v = A2[:].rearrange("p (a b) -> p a b", a=16); A4v = A4[:].rearrange("p (a b) -> p a b", a=16)
                    for sc in range(NSC):
                        c0 = sc * TS
                        if own:
                            psy, Bpsy = ybanks.get()
                        for t in range(4):
                            p0 = 4 * t
                            sl_ = slice(t * 512, (t + 1) * 512)
                            psr, Bpsr = banks.get(); psi, Bpsi = banks.get()
                            for pl in range(4):
                                S.op("pe", lambda e: e.matmul(psr[:, pl * TS:(pl + 1) * TS], lhsT=BL[:, p0 + pl, 0, :], rhs=uTb[:, t, c0:c0 + TS], start=True, stop=True), reads=[BBL, BuTb], writes=[Bpsr])
                            for pl in range(4):
                                S.op("pe", lambda e: e.matmul(psi[:, pl * TS:(pl + 1) * TS], lhsT=BL[:, p0 + pl, 1, :], rhs=uTb[:, t, c0:c0 + TS], start=True, stop=True), reads=[BBL, BuTb], writes=[Bpsi])
                            S.op("dve", lambda e: e.tensor_tensor(out=A1[:, sl_], in0=psr[:], in1=EcA[:, sl_], op=ALU.mult), reads=[Bpsr, BEc], writes=[BA1])
                            S.op("dve", lambda e: e.tensor_tensor(out=A2[:, sl_], in0=psi[:], in1=EsA[:, sl_], op=ALU.mult), reads=[Bpsi, BEs], writes=[BA2])
                            S.op("dve", lambda e: e.tensor_tensor(out=A3[:, sl_], in0=psi[:], in1=EcA[:, sl_], op=ALU.mult), reads=[Bpsi, BEc], writes=[BA3])
                            S.op("dve", lambda e: e.tensor_tensor(out=A4[:, sl_], in0=psr[:], in1=EsA[:, sl_], op=ALU.mult), reads=[Bpsr, BEs], writes=[BA4])
                            if own:
                                conv_fill(8)
                        S.op("dve", lambda e: e.tensor_tensor(out=A1[:], in0=A1[:], in1=A2[:], op=ALU.add), reads=[BA1, BA2], writes=[BA1])
                        S.op("dve", lambda e: e.tensor_tensor(out=A3[:], in0=A3[:], in1=A4[:], op=ALU.subtract), reads=[BA3, BA4], writes=[BA3])
                        S.op("dve", lambda e: e.tensor_tensor(out=st8[:, 0:16], in0=rho[:], in1=sre[:], op=ALU.mult), reads=[Brho, Bsre], writes=[Bst8])
                        S.op("dve", lambda e: e.tensor_tensor(out=st8[:, 16:32], in0=rho[:], in1=sim[:], op=ALU.mult), reads=[Brho, Bsim], writes=[Bst8])
                        S.op("dve", lambda e: e.tensor_tensor(out=A1v[:, :, 0], in0=A1v[:, :, 0], in1=st8[:, 0:16], op=ALU.add), reads=[BA1, Bst8], writes=[BA1])
                        S.op("dve", lambda e: e.tensor_tensor(out=A3v[:, :, 0], in0=A3v[:, :, 0], in1=st8[:, 16:32], op=ALU.add), reads=[BA3, Bst8], writes=[BA3])
                        if own:
                            conv_fill(16)
                        for t in range(4):
                            sl_ = slice(t * 512, (t + 1) * 512)
                            S.op("dve", lambda e: e.tensor_tensor_scan(out=A2[:, sl_], data0=R0A[:, sl_], data1=A1[:, sl_], initial=0.0, op0=ALU.mult, op1=ALU.add), reads=[Brho0, BA1], writes=[BA2])
                            S.op("dve", lambda e: e.tensor_tensor_scan(out=A4[:, sl_], data0=R0A[:, sl_], data1=A3[:, sl_], initial=0.0, op0=ALU.mult, op1=ALU.add), reads=[Brho0, BA3], writes=[BA4])
                        if own:
                            S.op("dve", lambda e: e.tensor_tensor(out=A1[:], in0=A2[:], in1=EcA, op=ALU.mult), reads=[BA2, BEc], writes=[BA1])
                            S.op("dve", lambda e: e.tensor_tensor(out=A3[:], in0=A4[:], in1=EsA, op=ALU.mult), reads=[BA4, BEs], writes=[BA3])
                            S.op("dve", lambda e: e.tensor_tensor(out=A2[:], in0=A2[:], in1=EsA, op=ALU.mult), reads=[BA2, BEs], writes=[BA2])
                            S.op("dve", lambda e: e.tensor_tensor(out=A4[:], in0=A4[:], in1=EcA, op=ALU.mult), reads=[BA4, BEc], writes=[BA4])
                            S.op("dve", lambda e: e.tensor_tensor(out=A1[:], in0=A1[:], in1=A3[:], op=ALU.subtract), reads=[BA1, BA3], writes=[BA1])
                            S.op("dve", lambda e: e.tensor_tensor(out=A4[:], in0=A4[:], in1=A2[:], op=ALU.add), reads=[BA4, BA2], writes=[BA4])
                            S.op("dve", lambda e: e.tensor_copy(out=sre[:], in_=A1v[:, :, TS - 1]), reads=[BA1], writes=[Bsre])
                            S.op("dve", lambda e: e.tensor_copy(out=sim[:], in_=A4v[:, :, TS - 1]), reads=[BA4], writes=[Bsim])
                            for t in range(4):
                                p0 = 4 * t
                                sl_ = slice(t * 512, (t + 1) * 512)
                                (xb1, Bxb1), (xb2, Bxb2) = xbr.get(), xbr.get()
                                S.op("act", lambda e: e.activation(out=xb1[:], in_=A1[:, sl_], func=AF.Identity), reads=[BA1], writes=[Bxb1])
                                S.op("act", lambda e: e.activation(out=xb2[:], in_=A4[:, sl_], func=AF.Identity), reads=[BA4], writes=[Bxb2])
                                for pl in range(4):
                                    S.op("pe", lambda e: e.matmul(psy[:, t * TS:(t + 1) * TS], lhsT=CL[:, p0 + pl, 0, :], rhs=xb1[:, pl * TS:(pl + 1) * TS], start=(pl == 0), stop=False), reads=[BCL, Bxb1], writes=[Bpsy])
                                    S.op("pe", lambda e: e.matmul(psy[:, t * TS:(t + 1) * TS], lhsT=CL[:, p0 + pl, 1, :], rhs=xb2[:, pl * TS:(pl + 1) * TS], start=False, stop=False), reads=[BCL, Bxb2], writes=[Bpsy])
                                S.op("pe", lambda e: e.matmul(psy[:, t * TS:(t + 1) * TS], lhsT=ddg[:, t, :], rhs=uTb[:, t, c0:c0 + TS], start=False, stop=True), reads=[Bddg, BuTb], writes=[Bpsy])
                            S.op("act", lambda e: e.activation(out=ys[:, :, c0:c0 + TS], in_=psy[:].rearrange("p (a b) -> p a b", a=4), func=AF.Identity), reads=[Bpsy], writes=[Bys])
                        else:
                            EcL = Ec[:, :, TS - 1]; EsL = Es[:, :, TS - 1]
                            v1L = A2v[:, :, TS - 1]; v2L = A4v[:, :, TS - 1]
                            S.op("dve", lambda e: e.tensor_tensor(out=st8[:, 32:48], in0=v1L, in1=EcL, op=ALU.mult), reads=[BA2, BEc], writes=[Bst8])
                            S.op("dve", lambda e: e.tensor_tensor(out=st8[:, 48:64], in0=v2L, in1=EsL, op=ALU.mult), reads=[BA4, BEs], writes=[Bst8])
                            S.op("dve", lambda e: e.tensor_tensor(out=sre[:], in0=st8[:, 32:48], in1=st8[:, 48:64], op=ALU.subtract), reads=[Bst8], writes=[Bsre])
                            S.op("dve", lambda e: e.tensor_tensor(out=st8[:, 32:48], in0=v2L, in1=EcL, op=ALU.mult), reads=[BA4, BEc], writes=[Bst8])
                            S.op("dve", lambda e: e.tensor_tensor(out=st8[:, 48:64], in0=v1L, in1=EsL, op=ALU.mult), reads=[BA2, BEs], writes=[Bst8])
                            S.op("dve", lambda e: e.tensor_tensor(out=sim[:], in0=st8[:, 32:48], in1=st8[:, 48:64], op=ALU.add), reads=[Bst8], writes=[Bsim])

                def inproj(chunks):
                    for oc in chunks:
                        ps, Bps = banks.get()
                        for k in range(8):
                            S.op("pe", lambda e, k=k: e.matmul(ps[:], lhsT=winb[:, k, oc * 128:(oc + 1) * 128], rhs=hT[:, k, :], start=(k == 0), stop=(k == 7)),
                                 reads=[Bwin, BhT], writes=[Bps])
                        yield oc, ps, Bps

                def front(xsrc, sti, own, need_z):
                    xts = []
                    for tl in range(4):
                        xt, Bxt = xr.get()
                        r0 = sti * 512 + tl * 128
                        S.dma("sp", xt[:], xsrc[r0:r0 + 128, :], writes=[Bxt])
                        norm_mod_T(xt, Bxt, 1024, 0, hT, BhT, tl)
                        xts.append((xt, Bxt))
                    for oc, ps, Bps in inproj(range(4)):
                        S.op("act", lambda e: e.activation(out=uTb[:, oc, :], in_=ps[:], func=AF.Identity), reads=[Bps], writes=[BuTb])
                    if need_z:
                        for c in range(4):
                            gen = inproj([8 + c, 4 + c])
                            _, psg, Bpsg = next(gen)
                            S.op("act", lambda e: e.activation(out=sg[:], in_=psg[:], func=AF.Sigmoid), reads=[Bpsg], writes=[Bsg])
                            _, psv, Bpsv = next(gen)
                            S.op("dve", lambda e: e.tensor_tensor(out=zbuf[:, c, 30:542], in0=psv[:], in1=sg[:], op=ALU.mult), reads=[Bpsv, Bsg], writes=[Bzbuf])
                    return xts

                for sti in range(NST - nprev, NST):
                    last = (sti == NST - 1)
                    issue_conv(6, pace=[BuTb])
                    front(x_prev, sti, False, last)
                    ssm(False)
                    if last:
                        S.op("dve", lambda e: e.tensor_scalar(out=zbuf[:, :, 0:30], in0=zbuf[:, :, 512:542], scalar1=flg[:, 0:1], scalar2=None, op0=ALU.mult),
                             reads=[Bzbuf, Bflg], writes=[Bzbuf])
                S.op("dve", lambda e: e.tensor_scalar(out=sre[:], in0=sre[:], scalar1=flg[:, 0:1], scalar2=None, op0=ALU.mult), reads=[Bsre, Bflg], writes=[Bsre])
                S.op("dve", lambda e: e.tensor_scalar(out=sim[:], in0=sim[:], scalar1=flg[:, 0:1], scalar2=None, op0=ALU.mult), reads=[Bsim, Bflg], writes=[Bsim])

                for sti in range(nst):
                    issue_conv(6, pace=[BuTb])
                    xts = front(x_own, sti, True, True)
                    conv_state["todo"] = [(c, j) for c in range(4) for j in range(31)]
                    ssm(True)
                    conv_fill(200)
                    S.op("act", lambda e: e.activation(out=yg[:], in_=ys[:], func=AF.Gelu), reads=[Bys], writes=[Byg])
                    for oc in range(4):
                        ps, Bps = banks.get()
                        for k in range(4):
                            S.op("pe", lambda e, k=k: e.matmul(ps[:], lhsT=wglub[:, k, oc * 128:(oc + 1) * 128], rhs=yg[:, k, :], start=(k == 0), stop=(k == 3)),
                                 reads=[Bwglu, Byg], writes=[Bps])
                        S.op("act", lambda e: e.activation(out=sg[:], in_=ps[:], func=AF.Sigmoid, bias=cols[:, oc:oc + 1]), reads=[Bps, Bcols], writes=[Bsg])
                        S.op("dve", lambda e: e.tensor_tensor(out=q[:, oc, :], in0=yg[:, oc, :], in1=sg[:], op=ALU.mult), reads=[Byg, Bsg], writes=[Bq])
                    S.op("act", lambda e: e.activation(out=q2[:], in_=q[:], func=AF.Square), reads=[Bq], writes=[Bq2])
                    ps, Bps = bsum_bc(q2, Bq2, 4)
                    r, Br = rstd_bc(ps, Bps, 512, 1e-6)
                    for oc in range(4):
                        S.op("dve", lambda e: e.scalar_tensor_tensor(out=mg_[:, oc, :], in0=q[:, oc, :], scalar=cols[:, 20 + oc:21 + oc], in1=r[:], op0=ALU.mult, op1=ALU.mult),
                             reads=[Bq, Bcols, Br], writes=[Bmg_])
                    S.op("dve", lambda e: e.tensor_copy(out=zbuf[:, :, 0:30], in_=zbuf[:, :, 512:542]), reads=[Bzbuf], writes=[Bzbuf])
                    ps1, Bps1 = bsum_bc(cv, Bcv, 4)
                    S.op("act", lambda e: e.activation(out=q2[:], in_=cv[:], func=AF.Square), reads=[Bcv], writes=[Bq2])
                    ps2, Bps2 = bsum_bc(q2, Bq2, 4)
                    mu, Bmu = nr_.get(); var, Bvar = nr_.get()
                    S.op("dve", lambda e: e.tensor_scalar(out=mu[:], in0=ps1[:], scalar1=1.0 / 512, scalar2=None, op0=ALU.mult), reads=[Bps1], writes=[Bmu])
                    S.op("dve", lambda e: e.tensor_tensor(out=var[:], in0=mu[:], in1=mu[:], op=ALU.mult), reads=[Bmu], writes=[Bvar])
                    S.op("dve", lambda e: e.scalar_tensor_tensor(out=var[:], in0=ps2[:], scalar=1.0 / 512, in1=var[:], op0=ALU.mult, op1=ALU.subtract), reads=[Bps2, Bvar], writes=[Bvar])
                    S.op("dve", lambda e: e.tensor_scalar(out=var[:], in0=var[:], scalar1=1e-5, scalar2=None, op0=ALU.add), reads=[Bvar], writes=[Bvar])
                    S.op("act", lambda e: e.activation(out=var[:], in_=var[:], func=AF.Sqrt), reads=[Bvar], writes=[Bvar])
                    S.op("dve", lambda e: e.reciprocal(out=var[:], in_=var[:]), reads=[Bvar], writes=[Bvar])
                    for c in range(4):
                        S.op("dve", lambda e: e.tensor_tensor(out=q[:, c, :], in0=cv[:, c, :], in1=mu[:], op=ALU.subtract), reads=[Bcv, Bmu], writes=[Bq])
                        S.op("dve", lambda e: e.tensor_tensor(out=q[:, c, :], in0=q[:, c, :], in1=var[:], op=ALU.mult), reads=[Bq, Bvar], writes=[Bq])
                        S.op("act", lambda e: e.activation(out=q[:, c, :], in_=q[:, c, :], func=AF.Silu, scale=cols[:, 12 + c:13 + c], bias=cols[:, 16 + c:17 + c]),
                             reads=[Bq, Bcols], writes=[Bq])
                    S.op("act", lambda e: e.activation(out=q2[:], in_=q[:], func=AF.Square), reads=[Bq], writes=[Bq2])
                    ps, Bps = bsum_bc(q2, Bq2, 4)
                    r, Br = rstd_bc(ps, Bps, 512, 1e-6)
                    for c in range(4):
                        S.op("dve", lambda e: e.scalar_tensor_tensor(out=mg_[:, 4 + c, :], in0=q[:, c, :], scalar=cols[:, 24 + c:25 + c], in1=r[:], op0=ALU.mult, op1=ALU.mult),
                             reads=[Bq, Bcols, Br], writes=[Bmg_])
                    for tl in range(4):
                        xt, Bxt = xr.get()
                        r0 = sti * 512 + tl * 128
                        S.dma("act", xt[:], x_own[r0:r0 + 128, :], writes=[Bxt])
                        x1t, Bx1t = x1r.get()
                        for nh in range(2):
                            ps, Bps = banks.get()
                            for k in range(8):
                                S.op("pe", lambda e, k=k: e.matmul(ps[:], lhsT=mg_[:, k, tl * 128:(tl + 1) * 128], rhs=woutb[:, k, nh * 512:(nh + 1) * 512], start=(k == 0), stop=(k == 7)),
                                     reads=[Bmg_, Bwout], writes=[Bps])
                            S.op("dve", lambda e: e.tensor_tensor(out=x1t[:, nh * 512:(nh + 1) * 512], in0=ps[:], in1=mod[:, 2048 + nh * 512:2048 + (nh + 1) * 512], op=ALU.mult),
                                 reads=[Bps, Bmod], writes=[Bx1t])
                        S.op("dve", lambda e: e.tensor_tensor(out=x1t[:], in0=x1t[:], in1=xt[:], op=ALU.add), reads=[Bx1t, Bxt], writes=[Bx1t])
                        r0 = sti * 512 + tl * 128
                        S.dma("sp", x1s[r0:r0 + 128, :], x1t[:], reads=[Bx1t], writes=[Bx1s[sti]], key=Bx1t)
                        norm_mod_T(x1t, Bx1t, 4096, 3072, h2T, Bh2T, tl, store=sti * 4 + tl)
                    for tl in range(4):
                        ps, Bps = banks.get()
                        for k in range(8):
                            S.op("pe", lambda e, k=k: e.matmul(ps[:, 0:32], lhsT=h2T[:, k, tl * 128:(tl + 1) * 128], rhs=rwb[:, k, :], start=(k == 0), stop=(k == 7)),
                                 reads=[Bh2T, Brw], writes=[Bps])
                        S.op("dve", lambda e: e.tensor_tensor(out=lg[:], in0=ps[:, 0:32], in1=rbb[:], op=ALU.add), reads=[Bps, Brbb], writes=[Blg])
                        S.op("dve", lambda e: e.max(out=mx8[:], in_=lg[:]), reads=[Blg], writes=[Bmx8])
                        S.op("dve", lambda e: e.tensor_scalar(out=msk[:], in0=lg[:], scalar1=mx8[:, 3:4], scalar2=None, op0=ALU.is_ge), reads=[Blg, Bmx8], writes=[Bmsk])
                        tix = sti * 4 + tl
                        S.op("pool", lambda e: e.tensor_copy(out=lgs[:, tix, :], in_=lg[:]), reads=[Blg], writes=[Blgs])
                        S.op("pool", lambda e: e.tensor_copy(out=mx4[:, tix, :], in_=mx8[:, 0:4]), reads=[Bmx8], writes=[Bmx4])
                        S.op("pool", lambda e: e.tensor_copy(out=mskb[:], in_=msk[:]), reads=[Bmsk], writes=[Bmskb])
                        psr, Bpsr = banks.get()
                        S.op("pe", lambda e: e.matmul(psr[:, 0:32], lhsT=ustb[:], rhs=mskb[:], start=True, stop=True), reads=[Bustb, Bmskb], writes=[Bpsr])
                        S.op("pe", lambda e: e.matmul(psr[:, 32:64], lhsT=onesb[:], rhs=mskb[:], start=True, stop=True), reads=[Bones, Bmskb], writes=[Bpsr])
                        S.op("dve", lambda e: e.tensor_tensor(out=rank[:, tix, :], in0=psr[:, 0:32], in1=cum[:, 0, :], op=ALU.add), reads=[Bpsr, Bcum], writes=[Brank])
                        S.op("dve", lambda e: e.tensor_tensor(out=cum[:, 0, :], in0=psr[:, 32:64], in1=cum[:, 0, :], op=ALU.add), reads=[Bpsr, Bcum], writes=[Bcum])
                        S.op("dve", lambda e: e.tensor_scalar(out=sm4[:, 0:1], in0=mx8[:, 0:1], scalar1=-1.0, scalar2=None, op0=ALU.mult), reads=[Bmx8], writes=[Bsm4])
                        S.op("act", lambda e: e.activation(out=lg[:], in_=lg[:], func=AF.Exp, bias=sm4[:, 0:1]), reads=[Blg, Bsm4], writes=[Blg])
                        S.op("dve", lambda e: e.tensor_tensor(out=lg[:], in0=lg[:], in1=msk[:], op=ALU.mult), reads=[Blg, Bmsk], writes=[Blg])
                        S.op("dve", lambda e: e.tensor_reduce(out=sm4[:, 1:2], in_=lg[:], axis=mybir.AxisListType.X, op=ALU.add), reads=[Blg], writes=[Bsm4])
                        S.op("dve", lambda e: e.reciprocal(out=sm4[:, 2:3], in_=sm4[:, 1:2]), reads=[Bsm4], writes=[Bsm4])
                        S.op("dve", lambda e: e.tensor_scalar(out=wts[:, sti * 4 + tl, :], in0=lg[:], scalar1=sm4[:, 2:3], scalar2=None, op0=ALU.mult), reads=[Blg, Bsm4], writes=[Bwts])
                        S.op("act", lambda e: e.activation(out=mx8[:, 4:8], in_=mx8[:, 0:4], func=AF.Exp, bias=sm4[:, 0:1]), reads=[Bmx8, Bsm4], writes=[Bmx8])
                        S.op("dve", lambda e: e.tensor_scalar(out=w4[:, tix, :], in0=mx8[:, 4:8], scalar1=sm4[:, 2:3], scalar2=None, op0=ALU.mult), reads=[Bmx8, Bsm4], writes=[Bw4])

        issue_conv(96)
        S.barrier()
        wg_rows = wgb.rearrange("e (p k) n -> (e p) (k n)", p=128)
        wu_rows = wub.rearrange("e (p k) n -> (e p) (k n)", p=128)
        wd_rows = wdb.rearrange("e (p k) n -> (e p) (k n)", p=128)
        with ExitStack() as me:
            desti, Bdesti = sbt(me, "desti", [128, 4, 32], I32)
            widx, Bwidx = sbt(me, "widx", [128, 2, NSLOT], I32)
            bidx2, Bbidx2 = sbt(me, "bidx2", [128, NSLOT], I32)
            with ExitStack() as dp:
                ci, Bci = sbt(dp, "ci", [128, 32], I32)
                pad, Bpad = sbt(dp, "pad", [128, 32], F32)
                on32, Bon32 = sbt(dp, "on32", [128, 32], F32)
                pend, Bpend = sbt(dp, "pend", [128, 1, 32], F32)
                pst_, Bpst_ = sbt(dp, "pst_", [128, 1, 32], F32)
                Dm, BDm = sbt(dp, "Dm", [128, 32, 32], F32)
                oh, Boh = sbt(dp, "oh", [128, 32, 32], F32)
                dstf, Bdstf = sbt(dp, "dstf", [128, 4, 32], F32)
                jr, Bjr = sbt(dp, "jr", [128, NSLOT, 1], F32)
                pc, Bpc = sbt(dp, "pc", [128, 3], F32)
                cmp_, Bcmp = sbt(dp, "cmp", [128, NSLOT, 32], F32)
                se, Bse = sbt(dp, "se", [128, NSLOT], F32)
                wf, Bwf = sbt(dp, "wf", [128, 2, NSLOT], F32)
                S.dma("sp", jr[:, :, 0], jr_c, writes=[Bjr]); S.dma("sp", pc[:], pcol_c, writes=[Bpc])
                NTH = NTOK // RS + 1
                thr, Bthr = sbt(dp, "thr", [128, 1, NTH], F32)
                cm2, Bcm2 = sbt(dp, "cm2", [128, 32, NTH], F32)
                nbl, Bnbl = sbt(dp, "nbl", [128, 32], F32)
                S.dma("sp", thr[:, 0, :], thr_c, writes=[Bthr])
                S.op("dve", lambda e: e.tensor_tensor(out=cm2[:], in0=thr[:].to_broadcast([128, 32, NTH]), in1=cum[:, 0, :].rearrange("p (e o) -> p e o", o=1).to_broadcast([128, 32, NTH]), op=ALU.is_lt),
                     reads=[Bthr, Bcum], writes=[Bcm2])
                S.op("dve", lambda e: e.tensor_reduce(out=nbl[:], in_=cm2[:], axis=mybir.AxisListType.X, op=ALU.add), reads=[Bcm2], writes=[Bnbl])
                S.op("dve", lambda e: e.tensor_scalar(out=pad[:], in0=nbl[:], scalar1=float(RS), scalar2=None, op0=ALU.mult), reads=[Bnbl], writes=[Bpad])
                S.op("dve", lambda e: e.tensor_copy(out=ci[:], in_=cum[:, 0, :]), reads=[Bcum], writes=[Bci])
                S.op("dve", lambda e: e.tensor_scalar(out=ci[:], in0=ci[:], scalar1=RS - 1, scalar2=None, op0=ALU.add), reads=[Bci], writes=[Bci])
                sh = int(math.log2(RS))
                S.op("dve", lambda e: e.tensor_scalar(out=ci[:], in0=ci[:], scalar1=sh, scalar2=sh, op0=ALU.arith_shift_right, op1=ALU.arith_shift_left), reads=[Bci], writes=[Bci])
                S.op("pool", lambda e: e.memset(on32[:], 1.0), writes=[Bon32])
                S.op("dve", lambda e: e.tensor_tensor_scan(out=pend[:, 0, :], data0=on32[:], data1=pad[:], initial=0.0, op0=ALU.mult, op1=ALU.add), reads=[Bon32, Bpad], writes=[Bpend])
                S.op("dve", lambda e: e.tensor_tensor(out=pst_[:, 0, :], in0=pend[:, 0, :], in1=pad[:], op=ALU.subtract), reads=[Bpend, Bpad], writes=[Bpst_])
                S.op("dve", lambda e: e.tensor_tensor(out=Dm[:], in0=rank[:], in1=pst_[:].to_broadcast([128, 32, 32]), op=ALU.add), reads=[Brank, Bpst_], writes=[BDm])
                for k in range(4):
                    S.op("dve", lambda e: e.tensor_tensor(out=oh[:], in0=lgs[:], in1=mx4[:, :, k:k + 1].to_broadcast([128, 32, 32]), op=ALU.is_equal), reads=[Blgs, Bmx4], writes=[Boh])
                    S.op("dve", lambda e: e.tensor_tensor(out=oh[:], in0=oh[:], in1=Dm[:], op=ALU.mult), reads=[Boh, BDm], writes=[Boh])
                    S.op("dve", lambda e: e.tensor_reduce(out=dstf[:, k, :], in_=oh[:], axis=mybir.AxisListType.X, op=ALU.add), reads=[Boh], writes=[Bdstf])
                S.op("dve", lambda e: e.tensor_copy(out=desti[:], in_=dstf[:]), reads=[Bdstf], writes=[Bdesti])
                S.op("dve", lambda e: e.tensor_tensor(out=cmp_[:], in0=pend[:].to_broadcast([128, NSLOT, 32]), in1=jr[:].to_broadcast([128, NSLOT, 32]), op=ALU.is_le), reads=[Bpend, Bjr], writes=[Bcmp])
                S.op("dve", lambda e: e.tensor_reduce(out=se[:], in_=cmp_[:], axis=mybir.AxisListType.X, op=ALU.add), reads=[Bcmp], writes=[Bse])
                S.op("dve", lambda e: e.tensor_scalar(out=se[:], in0=se[:], scalar1=31.0, scalar2=None, op0=ALU.min), reads=[Bse], writes=[Bse])
                S.op("dve", lambda e: e.tensor_scalar(out=wf[:, 0, :], in0=se[:], scalar1=128.0, scalar2=pc[:, 2:3], op0=ALU.mult, op1=ALU.add), reads=[Bse, Bpc], writes=[Bwf])
                S.op("dve", lambda e: e.tensor_copy(out=bidx2[:], in_=wf[:, 0, :]), reads=[Bwf], writes=[Bbidx2])
                for h in range(2):
                    S.op("dve", lambda e: e.tensor_scalar(out=wf[:, h, :], in0=se[:], scalar1=256.0, scalar2=pc[:, h:h + 1], op0=ALU.mult, op1=ALU.add), reads=[Bse, Bpc], writes=[Bwf])
                S.op("dve", lambda e: e.tensor_copy(out=widx[:], in_=wf[:]), reads=[Bwf], writes=[Bwidx])
                hrr = ring(dp, "hrr", [128, D], BF16, 4)
                scat_bufs = []
                Bscat = {}
                for t in range(nst * 4):
                    hrow, Bhrow = hrr.get()
                    S.dma("sp", hrow[:], h2rows[t * 128:(t + 1) * 128, :], reads=[Bh2r[t]], writes=[Bhrow])
                    for k in range(4):
                        S.idma(Xs, desti[:, k, t:t + 1], hrow[:], None, NSLOT * RS - 1, reads=[Bhrow, Bdesti], key=Bscat.setdefault(id(Bhrow), Buf(f"scat{len(Bscat)}")))
                    scat_bufs.append(Bhrow)
                BXs = Buf("Xs")
                S.wait_all("sp", [b for (_, b) in hrr.items])
                S.wait_all("pool", [b for (_, b) in hrr.items])

            S.barrier()
            nslot = NSLOT if nst == NST else max(4, (nst * 512 * 4) // RS + 32)
            bg_rows = b_gate_r.rearrange("e (p c) -> (e p) c", c=8)
            bu_rows = b_up_r.rearrange("e (p c) -> (e p) c", c=8)
            NR = RS // 128
            with ExitStack() as sl:
                wgr = ring(sl, "wg", [128, 8, D], BF16, 2)
                wur = ring(sl, "wu", [128, 8, D], BF16, 2)
                wdr = ring(sl, "wd", [128, 8, D], BF16, 2)
                bcr = ring(sl, "bc", [128, 16], F32, 3)
                xrr = ring(sl, "xrow", [128, D], BF16, 4)
                hTr = ring(sl, "hTm", [128, 8, RS], BF16, 2)
                aTr = ring(sl, "aT", [128, 8, RS], BF16, 2)
                glr = ring(sl, "gl", [128, RS], F32, 3)
                upr = ring(sl, "up", [128, RS], F32, 3)
                sgr = ring(sl, "sgm", [128, RS], F32, 3)
                yr = ring(sl, "yt", [128, D], F32, 3)
                rows_aps = (wg_rows, wu_rows, wd_rows)

                def issue_gathers(j):
                    st_ = {"w": (wgr.get(), wur.get(), wdr.get()), "bc": bcr.get()}
                    bc, Bbc = st_["bc"]
                    S.idma(bc[:, 0:8], None, bg_rows, bidx2[:, j:j + 1], 0, reads=[Bbidx2], writes=[Bbc], key=Bbc)
                    S.idma(bc[:, 8:16], None, bu_rows, bidx2[:, j:j + 1], 0, reads=[Bbidx2], writes=[Bbc], key=Bbc)
                    for mi in range(3):
                        wt_, Bwt_ = st_["w"][mi]
                        S.idma(wt_[:].rearrange("p k n -> p (k n)"), None, rows_aps[mi], bidx2[:, j:j + 1], 0, reads=[Bbidx2] + Bwconv_all, writes=[Bwt_], key=Bwt_)
                    return st_

                def load_rows(j):
                    hT, BhT = hTr.get()
                    for r in range(NR):
                        xrow, Bxrow = xrr.get()
                        r0 = j * RS + r * 128
                        S.dma("sp", xrow[:], Xs[r0:r0 + 128, :], writes=[Bxrow])
                        pst, Bpst = banks.get()
                        pv = pst[:].bitcast(BF16)
                        xv = xrow[:].rearrange("p (c k) -> p k c", k=8)
                        for k in range(8):
                            S.op("pe", lambda e: e.transpose(out=pv[:, k * 128:(k + 1) * 128], in_=xv[:, k, :], identity=idb[:]), reads=[Bxrow, Bidb], writes=[Bpst])
                        S.op("act", lambda e: e.activation(out=hT[:, :, r * 128:(r + 1) * 128], in_=pv[:, 0:1024].rearrange("p (k t) -> p k t", k=8), func=AF.Identity),
                             reads=[Bpst], writes=[BhT])
                    return hT, BhT

                cur = issue_gathers(0)
                cur_h = load_rows(0)
                for j in range(nslot):
                    nxt = issue_gathers(j + 1) if j + 1 < nslot else None
                    (wg, Bwg), (wu, Bwu), (wd, Bwd) = cur["w"]
                    bc, Bbc = cur["bc"]
                    S.op("dve", lambda e: e.tensor_scalar(out=bc[:, 8:16], in0=bc[:, 8:16], scalar1=1.0, scalar2=None, op0=ALU.add), reads=[Bbc], writes=[Bbc])
                    hT, BhT = cur_h
                    aT, BaT = aTr.get()
                    wgv = [wg[:, k, :].rearrange("p (m c) -> p c m", c=8) for k in range(8)]
                    wuv = [wu[:, k, :].rearrange("p (m c) -> p c m", c=8) for k in range(8)]
                    for fc in range(8):
                        psg, Bpsg = banks.get()
                        for k in range(8):
                            S.op("pe", lambda e: e.matmul(psg[:, 0:RS], lhsT=wgv[k][:, fc, :], rhs=hT[:, k, :], start=(k == 0), stop=(k == 7)), reads=[Bwg, BhT], writes=[Bpsg])
                        psu, Bpsu = banks.get()
                        for k in range(8):
                            S.op("pe", lambda e: e.matmul(psu[:, 0:RS], lhsT=wuv[k][:, fc, :], rhs=hT[:, k, :], start=(k == 0), stop=(k == 7)), reads=[Bwu, BhT], writes=[Bpsu])
                        gl, Bgl = glr.get(); up, Bup = upr.get(); sgm, Bsgm = sgr.get()
                        S.op("dve", lambda e: e.tensor_scalar(out=gl[:], in0=psg[:, 0:RS], scalar1=bc[:, fc:fc + 1], scalar2=7.0, op0=ALU.add, op1=ALU.min), reads=[Bpsg, Bbc], writes=[Bgl])
                        S.op("act", lambda e: e.activation(out=sgm[:], in_=gl[:], func=AF.Silu, scale=1.702), reads=[Bgl], writes=[Bsgm])
                        S.op("dve", lambda e: e.tensor_scalar(out=up[:], in0=psu[:, 0:RS], scalar1=bc[:, 8 + fc:9 + fc], scalar2=8.0, op0=ALU.add, op1=ALU.min), reads=[Bpsu, Bbc], writes=[Bup])
                        S.op("dve", lambda e: e.scalar_tensor_tensor(out=aT[:, fc, :], in0=up[:], scalar=-6.0, in1=sgm[:], op0=ALU.max, op1=ALU.mult), reads=[Bup, Bsgm], writes=[BaT])
                    if j + 1 < nslot:
                        cur_h = load_rows(j + 1)
                    for r in range(NR):
                        yt, Byt = yr.get()
                        for nh in range(2):
                            ps, Bps = banks.get()
                            for k in range(8):
                                S.op("pe", lambda e: e.matmul(ps[:], lhsT=aT[:, k, r * 128:(r + 1) * 128], rhs=wd[:, k, nh * 512:(nh + 1) * 512], start=(k == 0), stop=(k == 7)),
                                     reads=[BaT, Bwd], writes=[Bps])
                            S.op("act", lambda e: e.activation(out=yt[:, nh * 512:(nh + 1) * 512], in_=ps[:], func=AF.Copy, scale=1.0 / 1.702), reads=[Bps], writes=[Byt])
                        r0 = j * RS + r * 128
                        S.dma("sp", Ys[r0:r0 + 128, :], yt[:], reads=[Byt], key=Byt)
                    cur = nxt
                S.wait_all("pool", [b for (_, b) in yr.items])

            S.barrier()
            with ExitStack() as cb:
                ykr = ring(cb, "yk", [128, D], F32, 8)
                fgb2, Bfgb2 = sbt(cb, "fgb2", [128, D], F32)
                bdn, Bbdn = sbt(cb, "bdn", [32, D], F32)
                S.dma("sp", fgb2[:], fg_bc, writes=[Bfgb2]); S.dma("sp", bdn[:], b_down, writes=[Bbdn])
                acc, Bacc = sbt(cb, "acc", [128, D], F32)
                wT, BwT = sbt(cb, "wT", [32, 128], F32)
                x1r2 = ring(cb, "x1m", [128, D], F32, 2)
                sq2, Bsq2 = sbt(cb, "sq2", [128, D], F32)
                c4r = ring(cb, "c4", [128, 4], F32, 4)
                for t in range(nst * 4):
                    sti = t // 4
                    pst, Bpst = banks.get()
                    S.op("pe", lambda e: e.transpose(out=pst[0:32, 0:128], in_=wts[:, t, :], identity=idf[:]), reads=[Bwts, Bidf], writes=[Bpst])
                    S.op("act", lambda e: e.activation(out=wT[:], in_=pst[0:32, 0:128], func=AF.Identity), reads=[Bpst], writes=[BwT])
                    for nh in range(2):
                        ps, Bps = banks.get()
                        S.op("pe", lambda e: e.matmul(ps[:], lhsT=wT[:], rhs=bdn[:, nh * 512:(nh + 1) * 512], start=True, stop=True), reads=[BwT, Bbdn], writes=[Bps])
                        S.op("act", lambda e: e.activation(out=acc[:, nh * 512:(nh + 1) * 512], in_=ps[:], func=AF.Identity), reads=[Bps], writes=[Bacc])
                    for k in range(4):
                        yk, Byk = ykr.get()
                        S.idma(yk[:], None, Ys, desti[:, k, t:t + 1], NSLOT * RS - 1, reads=[Bdesti], writes=[Byk], key=Byk)
                        S.op("dve", lambda e: e.scalar_tensor_tensor(out=acc[:], in0=yk[:], scalar=w4[:, t, k:k + 1], in1=acc[:], op0=ALU.mult, op1=ALU.add),
                             reads=[Byk, Bw4, Bacc], writes=[Bacc])
                    x1t, Bx1t = x1r2.get()
                    r0 = t * 128
                    S.dma("sp", x1t[:], x1s[r0:r0 + 128, :], reads=[Bx1s[sti]], writes=[Bx1t])
                    S.op("dve", lambda e: e.tensor_tensor(out=acc[:], in0=acc[:], in1=g2b[:], op=ALU.mult), reads=[Bacc, Bg2b], writes=[Bacc])
                    S.op("dve", lambda e: e.tensor_tensor(out=x1t[:], in0=x1t[:], in1=acc[:], op=ALU.add), reads=[Bx1t, Bacc], writes=[Bx1t])
                    col, Bc = c4r.get()
                    S.op("act", lambda e: e.activation(out=sq2[:], in_=x1t[:], func=AF.Square, accum_out=col[:, 1:2]), reads=[Bx1t], writes=[Bsq2, Bc])
                    S.op("dve", lambda e: e.tensor_scalar(out=col[:, 2:3], in0=col[:, 1:2], scalar1=1.0 / D, scalar2=1e-6, op0=ALU.mult, op1=ALU.add), reads=[Bc], writes=[Bc])
                    S.op("act", lambda e: e.activation(out=col[:, 3:4], in_=col[:, 2:3], func=AF.Sqrt), reads=[Bc], writes=[Bc])
                    S.op("dve", lambda e: e.reciprocal(out=col[:, 0:1], in_=col[:, 3:4]), reads=[Bc], writes=[Bc])
                    S.op("dve", lambda e: e.scalar_tensor_tensor(out=x1t[:], in0=x1t[:], scalar=col[:, 0:1], in1=fgb2[:], op0=ALU.mult, op1=ALU.mult), reads=[Bx1t, Bc, Bfgb2], writes=[Bx1t])
                    S.dma("sp", out[r0:r0 + 128, :], x1t[:], reads=[Bx1t], key=Bx1t)
                S.wait_all("sp", [b for (_, b) in x1r2.items])
    return nc


_CFG = {"nst": NST, "nexp": NEXP, "nprev": NST}


def _host_inputs(inp):
    f = lambda a: np.ascontiguousarray(np.asarray(a, dtype=np.float32))
    x = f(inp["x"]); c = f(inp["c"])
    rep128 = lambda v: f(np.broadcast_to(np.asarray(v, np.float32).reshape(1, -1), (128, np.asarray(v).size)))
    col = lambda v, k: f(np.asarray(v, np.float32).reshape(k, 128).T)
    lam_re = f(inp["lam_re"][0]); lam_im = f(inp["lam_im"][0]); log_dt = f(inp["log_dt"][0])
    b_re = f(inp["b_re"][0]); b_im = f(inp["b_im"][0]); c_re = f(inp["c_re"][0]); c_im = f(inp["c_im"][0])
    def layB_lam(v):
        a = v.reshape(4, 8, 1, 64)
        return f(np.broadcast_to(a, (4, 8, 16, 64)).transpose(1, 2, 0, 3).reshape(128, 4, 64))
    def layB_b(v):
        return f(v.reshape(4, 8, 64, 16).transpose(1, 3, 0, 2).reshape(128, 4, 64))
    dt_full = np.broadcast_to(log_dt.reshape(32, 1), (32, 64))
    def layA_lam(v):
        return f(v.reshape(16, 2, 64).transpose(1, 2, 0).reshape(128, 16))
    def layA_c(v):
        return f(v.reshape(16, 2, 16, 64).transpose(1, 3, 0, 2).reshape(128, 16, 16))
    maskB = np.zeros((128, 8), np.float32)
    for gl in range(8):
        maskB[gl * 16:(gl + 1) * 16, gl] = 1.0
    shared = {
        "ada_w": f(inp["ada_w"][0]), "ada_b_bc": rep128(inp["ada_b"][0]),
        "n1g_bc": rep128(inp["norm1_g"][0]), "n2g_bc": rep128(inp["norm2_g"][0]), "fg_bc": rep128(inp["final_g"]),
        "w_in": f(inp["w_in"][0]), "w_out": f(inp["w_out"][0]), "w_glu": f(inp["w_glu"][0]),
        "router_w": f(inp["router_w"][0]), "router_b_bc": rep128(inp["router_b"][0]),
        "b_glu_col": col(inp["b_glu"][0], 4), "d_col": col(inp["d_skip"][0], 4), "conv_b_col": col(inp["conv_b"][0], 4),
        "ln_g_col": col(inp["ln_g"][0], 4), "ln_b_col": col(inp["ln_b"][0], 4), "og_col": col(inp["out_norm_g"][0], 8),
        "conv_wT": f(np.asarray(inp["conv_w"][0], np.float32).reshape(31, 4, 128).transpose(2, 1, 0)),
        "lamB_re": layB_lam(lam_re), "lamB_im": layB_lam(lam_im), "dtB": layB_lam(dt_full),
        "bB_re": layB_b(b_re), "bB_im": layB_b(b_im), "cA_re": layA_c(c_re), "cA_im": layA_c(c_im),
        "lamA_re": layA_lam(lam_re), "lamA_im": layA_lam(lam_im), "dtA": layA_lam(dt_full), "maskB": maskB,
        "b_gate_r": f(inp["b_gate"][0]), "b_up_r": f(inp["b_up"][0]),
        "ustrict": f(np.triu(np.ones((128, 128), np.float32), 1)),
        "thr_c": f(np.broadcast_to((np.arange(NTOK // RS + 1, dtype=np.float32) * RS).reshape(1, -1), (128, NTOK // RS + 1))),
        "jr_c": f(np.broadcast_to((np.arange(NSLOT, dtype=np.float32) * RS).reshape(1, -1), (128, NSLOT))),
        "pcol_c": f(np.stack([2.0 * np.arange(128), 2.0 * np.arange(128) + 1.0, 1.0 * np.arange(128)], axis=1)),
        "b_down": f(inp["b_down"][0]),
        "w_gate": f(inp["w_gate"][0]), "w_up": f(inp["w_up"][0]), "w_down": f(inp["w_down"][0]),
    }
    maps = []
    for core in range(8):
        b, hf = core // 2, core % 2
        m = dict(shared)
        m["x_own"] = f(x[b, hf * NTOK:(hf + 1) * NTOK])
        m["x_prev"] = f(x[b, 0:NTOK]) if hf == 1 else np.zeros((NTOK, D), np.float32)
        m["flag"] = np.full((128, 1), float(hf), np.float32)
        m["c_col"] = col(c[b], 8)
        maps.append(m)
    return maps


def kernel(**inputs):
    maps = _host_inputs(inputs)
    nc = build_nc(_CFG["nst"], _CFG["nexp"], _CFG["nprev"])
    res = run_bass_kernel_spmd(nc, maps, core_ids=list(range(8)))
    out = np.zeros((4, 2 * NTOK, D), np.float32)
    for core in range(8):
        b, hf = core // 2, core % 2
        out[b, hf * NTOK:(hf + 1) * NTOK] = np.asarray(res.results[core]["out"], np.float32)
    return out
```
